# Optimizing a Trainium2 kernel written in Bass

```python
import math
import jax, jax.numpy as jnp
from jax import lax
import numpy as np

D_MODEL = 1024
BATCH = 8
SEQ = 2048
DEPTH = 1

PLE_DIM = 256
EPS = 1e-6
ROPE_THETA = 10000.0
MOBA_HEADS = 8
MOBA_HEAD_DIM = 64
MOBA_BLOCK = 256
MOBA_TOPK = 3
MOBA_Q_CHUNK = 16
MOBA_WIDTH = MOBA_HEADS * MOBA_HEAD_DIM
MLA_HEADS = 8
MLA_Q_LORA = 256
MLA_KV_LORA = 128
MLA_NOPE_DIM = 64
MLA_ROPE_DIM = 32
MLA_V_DIM = 64
MLA_QK_DIM = MLA_NOPE_DIM + MLA_ROPE_DIM
MLA_WIDTH = MLA_HEADS * MLA_V_DIM
MLA_Q_BLOCK = 128
N_EXPERTS = 32
TOP_K = 4
D_EXPERT = 1024
SWIGLU_LIMIT = 7.0
SWIGLU_ALPHA = 1.702
EXPERT_ROW_BLOCK = 128
IN_SPLITS = (MOBA_WIDTH, MOBA_WIDTH, MOBA_WIDTH, MLA_Q_LORA, MLA_KV_LORA, MLA_ROPE_DIM, D_MODEL, D_MODEL)
D_IN = 3 * MOBA_WIDTH + MLA_Q_LORA + MLA_KV_LORA + MLA_ROPE_DIM + 2 * D_MODEL

kernel_name = "hybrid_moba_mla_moe_ple_block"


def rmsnorm(x, g):
    xf = x.astype(jnp.float32)
    y = xf * lax.rsqrt(jnp.mean(xf * xf, axis=-1, keepdims=True) + EPS)
    return (y * g.astype(jnp.float32)).astype(x.dtype)


def apply_rope(x):
    s, dim = x.shape[-2], x.shape[-1]
    half = dim // 2
    inv_freq = ROPE_THETA ** (-(jnp.arange(half, dtype=jnp.float32) / half))
    ang = jnp.arange(s, dtype=jnp.float32)[:, None] * inv_freq[None, :]
    cos, sin = jnp.cos(ang), jnp.sin(ang)
    xf = x.astype(jnp.float32)
    x1, x2 = xf[..., :half], xf[..., half:]
    return jnp.concatenate([x1 * cos - x2 * sin, x2 * cos + x1 * sin], axis=-1).astype(x.dtype)


def moba_attention(q, k, v):
    b, h, s, hd = q.shape
    nb = -(-s // MOBA_BLOCK)
    s_pad = nb * MOBA_BLOCK
    padcfg = ((0, 0), (0, 0), (0, s_pad - s), (0, 0))
    q, k, v = jnp.pad(q, padcfg), jnp.pad(k, padcfg), jnp.pad(v, padcfg)
    scale = hd ** -0.5
    q_blk = q.reshape(b, h, nb, MOBA_BLOCK, hd)
    k_blk = k.reshape(b, h, nb, MOBA_BLOCK, hd)
    v_blk = v.reshape(b, h, nb, MOBA_BLOCK, hd)
    k_mean = jnp.mean(k_blk.astype(jnp.float32), axis=3)
    gate = jnp.einsum('bhsd,bhnd->bhsn', q.astype(jnp.float32), k_mean)
    cur_blk = jnp.arange(s_pad) // MOBA_BLOCK
    is_past = jnp.arange(nb)[None, :] < cur_blk[:, None]
    gate = jnp.where(is_past, gate, -jnp.inf)
    n_sel = min(MOBA_TOPK, nb)
    _, sel = lax.top_k(gate, n_sel)
    sel_valid = sel < cur_blk[:, None]
    s_own = jnp.einsum('bhnqd,bhnkd->bhnqk', q_blk, k_blk) * scale
    causal = jnp.tril(jnp.ones((MOBA_BLOCK, MOBA_BLOCK), dtype=bool))
    s_own = jnp.where(causal, s_own, -jnp.inf).reshape(b, h, s_pad, MOBA_BLOCK)
    gather_blocks = jax.vmap(jax.vmap(lambda blocks, ix: blocks[ix]))

    def chunk(ci):
        start = ci * MOBA_Q_CHUNK
        qc = lax.dynamic_slice_in_dim(q, start, MOBA_Q_CHUNK, axis=2)
        ix = lax.dynamic_slice_in_dim(sel, start, MOBA_Q_CHUNK, axis=2)
        ok = lax.dynamic_slice_in_dim(sel_valid, start, MOBA_Q_CHUNK, axis=2)
        so = lax.dynamic_slice_in_dim(s_own, start, MOBA_Q_CHUNK, axis=2)
        v_own = lax.dynamic_index_in_dim(v_blk, start // MOBA_BLOCK, axis=2, keepdims=False)
        k_sel = gather_blocks(k_blk, ix)
        v_sel = gather_blocks(v_blk, ix)
        ss = jnp.einsum('bhqd,bhqjkd->bhqjk', qc, k_sel) * scale
        ss = jnp.where(ok[..., None], ss, -jnp.inf)
        logits = jnp.concatenate([so, ss.reshape(b, h, MOBA_Q_CHUNK, n_sel * MOBA_BLOCK)], axis=-1)
        probs = jax.nn.softmax(logits.astype(jnp.float32), axis=-1).astype(v.dtype)
        p_own = probs[..., :MOBA_BLOCK]
        p_sel = probs[..., MOBA_BLOCK:].reshape(b, h, MOBA_Q_CHUNK, n_sel, MOBA_BLOCK)
        return (jnp.einsum('bhqk,bhkd->bhqd', p_own, v_own)
                + jnp.einsum('bhqjk,bhqjkd->bhqd', p_sel, v_sel))

    out = lax.map(chunk, jnp.arange(s_pad // MOBA_Q_CHUNK))
    out = jnp.moveaxis(out, 0, 2).reshape(b, h, s_pad, hd)
    return out[:, :, :s]


def causal_attention(q, k, v):
    b, h, s, dq = q.shape
    scale = dq ** -0.5
    kpos = jnp.arange(s)

    def blk(ci):
        start = ci * MLA_Q_BLOCK
        qc = lax.dynamic_slice_in_dim(q, start, MLA_Q_BLOCK, axis=2)
        sc = jnp.einsum('bhqd,bhkd->bhqk', qc, k).astype(jnp.float32) * scale
        qpos = start + jnp.arange(MLA_Q_BLOCK)
        sc = jnp.where(kpos[None, :] <= qpos[:, None], sc, -jnp.inf)
        pr = jax.nn.softmax(sc, axis=-1).astype(v.dtype)
        return jnp.einsum('bhqk,bhkd->bhqd', pr, v)

    out = lax.map(blk, jnp.arange(s // MLA_Q_BLOCK))
    return jnp.moveaxis(out, 0, 2).reshape(b, h, s, v.shape[-1])


def routed_experts(h, w_router, b_router, w_gate_up, b_gate_up, w_down, b_down):
    b, s, d = h.shape
    n = b * s
    hf = h.reshape(n, d)
    logits = (hf @ w_router).astype(jnp.float32) + b_router.astype(jnp.float32)
    top_vals, top_idx = lax.top_k(logits, TOP_K)
    top_w = jax.nn.softmax(top_vals, axis=-1)
    nk = n * TOP_K
    m = EXPERT_ROW_BLOCK
    flat_e = top_idx.reshape(nk)
    flat_t = jnp.arange(nk, dtype=jnp.int32) // TOP_K
    flat_w = top_w.reshape(nk)
    order = jnp.argsort(flat_e)
    se = flat_e[order]
    counts = jnp.bincount(flat_e, length=N_EXPERTS)
    starts = jnp.cumsum(counts) - counts
    pcounts = ((counts + m - 1) // m) * m
    pends = jnp.cumsum(pcounts)
    pstarts = pends - pcounts
    dest = pstarts[se] + (jnp.arange(nk) - starts[se])
    n_rows = ((nk + m - 1) // m) * m + N_EXPERTS * m
    n_blocks = n_rows // m
    row_tok = jnp.full((n_rows,), n, dtype=jnp.int32).at[dest].set(flat_t[order])
    row_w = jnp.zeros((n_rows,), jnp.float32).at[dest].set(flat_w[order])
    blk_e = jnp.clip(jnp.searchsorted(pends, jnp.arange(n_blocks) * m, side='right'), 0, N_EXPERTS - 1)
    h_pad = jnp.concatenate([hf, jnp.zeros((1, d), hf.dtype)], axis=0)
    xr = h_pad[row_tok].reshape(n_blocks, m, d)

    def expert_block(args):
        xb, e = args
        gu = xb @ w_gate_up[e] + b_gate_up[e]
        g, u = gu[:, :D_EXPERT], gu[:, D_EXPERT:]
        g = jnp.minimum(g, SWIGLU_LIMIT)
        u = jnp.clip(u, -SWIGLU_LIMIT, SWIGLU_LIMIT)
        glu = g * jax.nn.sigmoid(SWIGLU_ALPHA * g)
        return ((u + 1.0) * glu) @ w_down[e] + b_down[e]

    yr = lax.map(expert_block, (xr, blk_e)).reshape(n_rows, d)
    y = jax.ops.segment_sum(yr * row_w[:, None].astype(yr.dtype), row_tok, num_segments=n + 1)[:n]
    return y.reshape(b, s, d).astype(h.dtype)


def hybrid_layer(x, p_i, g_mix, w_in, moba_q_norm, moba_k_norm, mla_q_lat_norm, w_uq,
                 mla_kv_lat_norm, w_ukv, mla_q_norm, mla_k_norm, w_branch_a, w_branch_b, w_out,
                 g_ffn, w_router, b_router, w_gate_up, b_gate_up, w_down, b_down,
                 g_ple, w_ple_gate, w_ple_proj):
    b, s, d = x.shape
    hn = rmsnorm(x, g_mix)
    proj = hn @ w_in
    parts, off = [], 0
    for wdt in IN_SPLITS:
        parts.append(proj[..., off:off + wdt])
        off += wdt
    q_a, k_a, v_a, c_q, c_kv, k_pe, gate_a, gate_b = parts

    heads_a = lambda t: t.reshape(b, s, MOBA_HEADS, MOBA_HEAD_DIM).transpose(0, 2, 1, 3)
    q_a = apply_rope(rmsnorm(heads_a(q_a), moba_q_norm))
    k_a = apply_rope(rmsnorm(heads_a(k_a), moba_k_norm))
    y_a = moba_attention(q_a, k_a, heads_a(v_a))
    y_a = y_a.transpose(0, 2, 1, 3).reshape(b, s, MOBA_WIDTH)

    q_b = (rmsnorm(c_q, mla_q_lat_norm) @ w_uq).reshape(b, s, MLA_HEADS, MLA_QK_DIM).transpose(0, 2, 1, 3)
    kv = (rmsnorm(c_kv, mla_kv_lat_norm) @ w_ukv).reshape(b, s, MLA_HEADS, MLA_NOPE_DIM + MLA_V_DIM).transpose(0, 2, 1, 3)
    k_nope, v_b = kv[..., :MLA_NOPE_DIM], kv[..., MLA_NOPE_DIM:]
    k_pe_h = jnp.broadcast_to(k_pe[:, None], (b, MLA_HEADS, s, MLA_ROPE_DIM))
    k_b = jnp.concatenate([k_nope, k_pe_h], axis=-1)
    q_b = rmsnorm(q_b, mla_q_norm)
    k_b = rmsnorm(k_b, mla_k_norm)
    q_b = jnp.concatenate([q_b[..., :MLA_NOPE_DIM], apply_rope(q_b[..., MLA_NOPE_DIM:])], axis=-1)
    k_b = jnp.concatenate([k_b[..., :MLA_NOPE_DIM], apply_rope(k_b[..., MLA_NOPE_DIM:])], axis=-1)
    y_b = causal_attention(q_b, k_b, v_b)
    y_b = y_b.transpose(0, 2, 1, 3).reshape(b, s, MLA_WIDTH)

    merged = jax.nn.sigmoid(gate_a) * (y_a @ w_branch_a) + jax.nn.sigmoid(gate_b) * (y_b @ w_branch_b)
    x = x + merged @ w_out

    x = x + routed_experts(rmsnorm(x, g_ffn), w_router, b_router, w_gate_up, b_gate_up, w_down, b_down)

    hp = rmsnorm(x, g_ple)
    x = x + jax.nn.sigmoid(hp @ w_ple_gate) * (p_i @ w_ple_proj)
    return x


def setup_inputs(seed: int = 0) -> dict:
    key = jax.random.key(seed)
    ks = jax.random.split(key, 32)
    f32 = jnp.float32

    def nrm(k, shape, scale):
        return jax.random.normal(k, shape, f32) * scale

    def gain(k, dim):
        return 1.0 + 0.02 * jax.random.normal(k, (DEPTH, dim), f32)

    L = DEPTH
    return {
        "x": nrm(ks[0], (BATCH, SEQ, D_MODEL), 1.0),
        "p": nrm(ks[1], (DEPTH, BATCH, SEQ, PLE_DIM), 1.0),
        "g_mix": gain(ks[2], D_MODEL),
        "w_in": nrm(ks[3], (L, D_MODEL, D_IN), D_MODEL ** -0.5),
        "moba_q_norm": gain(ks[4], MOBA_HEAD_DIM),
        "moba_k_norm": gain(ks[5], MOBA_HEAD_DIM),
        "mla_q_lat_norm": gain(ks[6], MLA_Q_LORA),
        "w_uq": nrm(ks[7], (L, MLA_Q_LORA, MLA_HEADS * MLA_QK_DIM), MLA_Q_LORA ** -0.5),
        "mla_kv_lat_norm": gain(ks[8], MLA_KV_LORA),
        "w_ukv": nrm(ks[9], (L, MLA_KV_LORA, MLA_HEADS * (MLA_NOPE_DIM + MLA_V_DIM)), MLA_KV_LORA ** -0.5),
        "mla_q_norm": gain(ks[10], MLA_QK_DIM),
        "mla_k_norm": gain(ks[11], MLA_QK_DIM),
        "w_branch_a": nrm(ks[12], (L, MOBA_WIDTH, D_MODEL), MOBA_WIDTH ** -0.5),
        "w_branch_b": nrm(ks[13], (L, MLA_WIDTH, D_MODEL), MLA_WIDTH ** -0.5),
        "w_out": nrm(ks[14], (L, D_MODEL, D_MODEL), D_MODEL ** -0.5),
        "g_ffn": gain(ks[15], D_MODEL),
        "w_router": nrm(ks[16], (L, D_MODEL, N_EXPERTS), D_MODEL ** -0.5),
        "b_router": nrm(ks[17], (L, N_EXPERTS), 0.01),
        "w_gate_up": nrm(ks[18], (L, N_EXPERTS, D_MODEL, 2 * D_EXPERT), D_MODEL ** -0.5),
        "b_gate_up": nrm(ks[19], (L, N_EXPERTS, 2 * D_EXPERT), 0.01),
        "w_down": nrm(ks[20], (L, N_EXPERTS, D_EXPERT, D_MODEL), D_EXPERT ** -0.5),
        "b_down": nrm(ks[21], (L, N_EXPERTS, D_MODEL), 0.01),
        "g_ple": gain(ks[22], D_MODEL),
        "w_ple_gate": nrm(ks[23], (L, D_MODEL, D_MODEL), D_MODEL ** -0.5),
        "w_ple_proj": nrm(ks[24], (L, PLE_DIM, D_MODEL), PLE_DIM ** -0.5),
    }


def reference(x, p, g_mix, w_in, moba_q_norm, moba_k_norm, mla_q_lat_norm, w_uq,
              mla_kv_lat_norm, w_ukv, mla_q_norm, mla_k_norm, w_branch_a, w_branch_b, w_out,
              g_ffn, w_router, b_router, w_gate_up, b_gate_up, w_down, b_down,
              g_ple, w_ple_gate, w_ple_proj):
    for i in range(DEPTH):
        x = hybrid_layer(x, p[i], g_mix[i], w_in[i], moba_q_norm[i], moba_k_norm[i],
                         mla_q_lat_norm[i], w_uq[i], mla_kv_lat_norm[i], w_ukv[i],
                         mla_q_norm[i], mla_k_norm[i], w_branch_a[i], w_branch_b[i], w_out[i],
                         g_ffn[i], w_router[i], b_router[i], w_gate_up[i], b_gate_up[i],
                         w_down[i], b_down[i], g_ple[i], w_ple_gate[i], w_ple_proj[i])
    return x
```

```python
import contextlib
import math
import numpy as np
import concourse.bass as bass
import concourse.mybir as mybir
from concourse.bass_utils import run_bass_kernel_spmd

F32 = mybir.dt.float32
BF16 = mybir.dt.bfloat16
ALU = mybir.AluOpType
ACT = mybir.ActivationFunctionType
AX = mybir.AxisListType

ENGS = ("pe", "act", "dve", "pool", "sp")
EPS = 1e-6
NEG = -30000.0
D = 1024
NEXP = 32


class _Op:
    __slots__ = ("eng", "fn", "dma", "dsem", "deps", "sig", "has_dep")

    def __init__(self, eng, fn, dma, dsem):
        self.eng = eng
        self.fn = fn
        self.dma = dma
        self.dsem = dsem
        self.deps = []
        self.sig = None
        self.has_dep = False


class _Res:
    __slots__ = ("w", "readers")

    def __init__(self):
        self.w = None
        self.readers = {}


class Prog:
    def __init__(self, nc):
        self.nc = nc
        self.ops = []
        self.res = {}
        self.last_dma_on_sem = {}
        self.final_ops = []
        self.last_on_eng = {}
        self.pending_dma = {}
        self.rec = None

    def op(self, eng, fn, reads=(), writes=(), dma=False, dsem=None, final=False, extra_deps=()):
        if self.rec is not None:
            self.rec.append((eng, fn, tuple(reads), tuple(writes), dma, dsem, final))
            return None
        o = _Op(eng, fn, dma, dsem)
        deps = {}

        def add(d, raw):
            if d is None or d is o:
                return
            if (not d.dma) and (not dma) and d.eng == eng:
                if not raw:
                    return
                if eng == "pe":
                    return
            deps[id(d)] = d

        for r in reads:
            st = self.res.get(r)
            if st is not None:
                add(st.w, True)
        for w in writes:
            st = self.res.get(w)
            if st is None:
                st = self.res[w] = _Res()
            add(st.w, False)
            for rd in st.readers.values():
                add(rd, False)
        for d in extra_deps:
            deps[id(d)] = d
        if dma:
            prev = self.last_dma_on_sem.get(dsem)
            if prev is not None:
                deps[id(prev)] = prev
            self.last_dma_on_sem[dsem] = o
            self.pending_dma[dsem] = o
        o.deps = list(deps.values())
        for d in o.deps:
            d.has_dep = True
        rk = ("d", dsem) if dma else ("e", eng)
        for r in reads:
            st = self.res.get(r)
            if st is None:
                st = self.res[r] = _Res()
            st.readers[rk] = o
        for w in writes:
            st = self.res[w]
            st.w = o
            st.readers = {}
        if final:
            o.has_dep = True
            self.final_ops.append(o)
        if not dma:
            self.last_on_eng[eng] = o
        self.ops.append(o)
        return o

    def interleave(self, fn, idxs, group=2):
        idxs = list(idxs)
        for g0 in range(0, len(idxs), group):
            lists = []
            for i in idxs[g0:g0 + group]:
                self.rec = []
                fn(i)
                lists.append(self.rec)
                self.rec = None
            pos = [0] * len(lists)
            left = sum(len(l) for l in lists)
            while left:
                for k, l in enumerate(lists):
                    if pos[k] < len(l):
                        eng, f, r, w, dma, dsem, final = l[pos[k]]
                        pos[k] += 1
                        left -= 1
                        self.op(eng, f, reads=r, writes=w, dma=dma, dsem=dsem, final=final)

    def barrier(self):
        tails = list(self.last_on_eng.values()) + list(self.pending_dma.values())
        for e in ENGS:
            self.op(e, lambda h: h.nop(), extra_deps=[t for t in tails])
        self.res = {}

    def emit(self):
        nc = self.nc
        with contextlib.ExitStack() as es:
            SEM_MAX = 4000
            nsig = {e: 0 for e in ENGS}
            for o in self.ops:
                if (not o.dma) and o.has_dep:
                    nsig[o.eng] += 1
            esem = {e: [es.enter_context(nc.semaphore("s_%s%d" % (e, k))) for k in range(nsig[e] // SEM_MAX + 1)]
                    for e in ENGS}
            dsems = {}
            for o in self.ops:
                if o.dma and o.dsem not in dsems:
                    dsems[o.dsem] = es.enter_context(nc.semaphore("d%d" % len(dsems)))
            ecount = {e: 0 for e in ENGS}
            dcount = {k: 0 for k in dsems}
            for o in self.ops:
                if o.dma:
                    dcount[o.dsem] += 16
                    o.sig = (dsems[o.dsem], dcount[o.dsem])
                elif o.has_dep:
                    c0 = ecount[o.eng]
                    ecount[o.eng] += 1
                    o.sig = (esem[o.eng][c0 // SEM_MAX], c0 % SEM_MAX + 1)
            block = es.enter_context(nc.Block())
            per_eng = {e: [o for o in self.ops if o.eng == e] for e in ENGS}
            final_ops = self.final_ops

            def run(e, handle):
                waited = {}
                for o in per_eng[e]:
                    for d in o.deps:
                        sem, val = d.sig
                        k = id(sem)
                        if waited.get(k, 0) < val:
                            handle.wait_ge(sem, val)
                            waited[k] = val
                    ins = o.fn(handle)
                    if o.sig is not None:
                        ins.then_inc(o.sig[0], 16 if o.dma else 1)
                if e == "sp":
                    for o in final_ops:
                        sem, val = o.sig
                        if waited.get(id(sem), 0) < val:
                            handle.wait_ge(sem, val)
                            waited[id(sem)] = val

            @block.tensor
            def _(h):
                run("pe", h)

            @block.scalar
            def _(h):
                run("act", h)

            @block.vector
            def _(h):
                run("dve", h)

            @block.gpsimd
            def _(h):
                run("pool", h)

            @block.sync
            def _(h):
                run("sp", h)


class Arena:
    def __init__(self, t, n):
        self.t = t
        self.n = n
        self.off = 0

    def alloc(self, shape, dtype=BF16):
        assert shape[0] == 128
        nel = 1
        for s in shape[1:]:
            nel *= s
        esz = 2 if dtype == BF16 else 4
        n16 = (nel * esz + 1) // 2
        n16 = (n16 + 31) // 32 * 32
        start = self.off
        self.off += n16
        assert self.off <= self.n, "arena overflow %d > %d" % (self.off, self.n)
        v = self.t[:, start:start + (nel * esz) // 2]
        if dtype != BF16:
            v = v.bitcast(dtype)
        if len(shape) > 2:
            names = " ".join("d%d" % k for k in range(len(shape) - 1))
            kw = {"d%d" % k: shape[k + 1] for k in range(len(shape) - 1)}
            v = v.rearrange("p (%s) -> p %s" % (names, names), **kw)
        return v

    def mark(self):
        return self.off

    def reset(self, m):
        self.off = m


def bc(ap, axis, shape):
    return ap.unsqueeze(axis).to_broadcast(list(shape))


def build_nc(S, n_exp=NEXP, debug=False):
    NT = S // 128
    NQ = S // 512
    NB = S // 256
    nc = bass.Bass("TRN2", target_bir_lowering=False)
    din = lambda name, shape: nc.dram_tensor(name, shape, F32, kind="ExternalInput").ap()
    x = din("x", [S, D])
    p_in = din("p", [S, 256])
    g_mix = din("g_mix", [1, D])
    w_in = din("w_in", [D, 4000])
    moba_q_norm = din("moba_q_norm", [1, 64])
    moba_k_norm = din("moba_k_norm", [1, 64])
    mla_q_lat_norm = din("mla_q_lat_norm", [1, 256])
    w_uq = din("w_uq", [256, 768])
    mla_kv_lat_norm = din("mla_kv_lat_norm", [1, 128])
    w_ukv = din("w_ukv", [128, 1024])
    mla_q_norm = din("mla_q_norm", [1, 96])
    mla_k_norm = din("mla_k_norm", [1, 96])
    w_branch_a = din("w_branch_a", [512, D])
    w_branch_b = din("w_branch_b", [512, D])
    w_out = din("w_out", [D, D])
    g_ffn = din("g_ffn", [1, D])
    w_router = din("w_router", [D, NEXP])
    b_router = din("b_router", [1, NEXP])
    w_gate_up = din("w_gate_up", [NEXP, D, 2048])
    b_gate_up = din("b_gate_up", [NEXP, 2048])
    w_down = din("w_down", [NEXP, D, D])
    b_down = din("b_down", [NEXP, D])
    g_ple = din("g_ple", [1, D])
    w_ple_gate = din("w_ple_gate", [D, D])
    w_ple_proj = din("w_ple_proj", [256, D])
    c_ident = din("c_ident", [128, 128])
    c_tri = din("c_tri", [128, 128])
    c_triu = din("c_triu", [128, 128])
    c_iota = din("c_iota", [1, 128])
    c_cosA = din("c_cosA", [S, 32])
    c_sinA = din("c_sinA", [S, 32])
    c_cosB = din("c_cosB", [S, 16])
    c_sinB = din("c_sinB", [S, 16])
    out = nc.dram_tensor("out", [S, D], F32, kind="ExternalOutput").ap()
    dbg = {}
    if debug:
        dbg["x1"] = nc.dram_tensor("dbg_x1", [S, D], F32, kind="ExternalOutput").ap()
        dbg["x2"] = nc.dram_tensor("dbg_x2", [S, D], F32, kind="ExternalOutput").ap()
        dbg["ya"] = nc.dram_tensor("dbg_ya", [512, S], F32, kind="ExternalOutput").ap()
        dbg["yb"] = nc.dram_tensor("dbg_yb", [512, S], F32, kind="ExternalOutput").ap()

    P = Prog(nc)
    AR_N = 99 * 1024 + 512
    with contextlib.ExitStack() as es:
        arena_t = es.enter_context(nc.sbuf_tensor("arena", [128, AR_N], BF16))
        PS = es.enter_context(nc.psum_tensor("ps", [128, 8, 512], F32))
        A = Arena(arena_t, AR_N)

        def psb(b):
            return PS[:, b, :]

        def pst(b):
            return PS[:, b, :].bitcast(BF16).rearrange("p (a b) -> p a b", a=8)

        dve = lambda fn, r, w: P.op("dve", fn, reads=r, writes=w)
        act = lambda fn, r, w: P.op("act", fn, reads=r, writes=w)
        pool = lambda fn, r, w: P.op("pool", fn, reads=r, writes=w)
        pe = lambda fn, r, w: P.op("pe", fn, reads=r, writes=w)
        _dq = [0]

        def dma(out_ap, in_ap, r, w, key, q=None, final=False):
            if q is None:
                q = "sp"
            return P.op(q, lambda e: e.dma_start(out=out_ap, in_=in_ap), reads=r, writes=w, dma=True,
                        dsem=key, final=final)

        def dma_cast(out_ap, in_ap, r, w, key):
            return P.op("pool", lambda e: e.dma_start(out=out_ap, in_=in_ap), reads=r, writes=w, dma=True, dsem=key)

        identf = A.alloc([128, 128], F32)
        identb = A.alloc([128, 128], BF16)
        g_bc = A.alloc([128, D], F32)
        stat = A.alloc([128, 64], F32)
        tmpf = A.alloc([128, 128], F32)
        m0 = A.mark()
        trib = A.alloc([128, 128], BF16)
        cosA = A.alloc([128, NT, 32], F32)
        sinA = A.alloc([128, NT, 32], F32)
        cosB = A.alloc([128, NT, 16], F32)
        sinB = A.alloc([128, NT, 16], F32)
        gqa = A.alloc([128, 64], F32)
        gka = A.alloc([128, 64], F32)
        gql = A.alloc([128, 256], F32)
        gkvl = A.alloc([128, 128], F32)
        gqb = A.alloc([128, 96], F32)
        gkb = A.alloc([128, 96], F32)

        dma(identf, c_ident, [], ["identf"], "c0")
        dma(tmpf, c_tri, [], ["tmpf"], "c1")
        dve(lambda e: e.tensor_copy(out=identb, in_=identf), ["identf"], ["identb"])
        dve(lambda e: e.tensor_copy(out=trib, in_=tmpf), ["tmpf"], ["trib"])
        dma(cosA, c_cosA.rearrange("(t p) d -> p t d", p=128), [], ["cosA"], "c2")
        dma(sinA, c_sinA.rearrange("(t p) d -> p t d", p=128), [], ["sinA"], "c3")
        dma(cosB, c_cosB.rearrange("(t p) d -> p t d", p=128), [], ["cosB"], "c4")
        dma(sinB, c_sinB.rearrange("(t p) d -> p t d", p=128), [], ["sinB"], "c5")
        dma(g_bc, g_mix.to_broadcast([128, D]), [], ["g_bc"], "c6")
        dma(gqa, moba_q_norm.to_broadcast([128, 64]), [], ["gqa"], "c7")
        dma(gka, moba_k_norm.to_broadcast([128, 64]), [], ["gka"], "c8")
        dma(gql, mla_q_lat_norm.to_broadcast([128, 256]), [], ["gql"], "c9")
        dma(gkvl, mla_kv_lat_norm.to_broadcast([128, 128]), [], ["gkvl"], "c10")
        dma(gqb, mla_q_norm.to_broadcast([128, 96]), [], ["gqb"], "c11")
        dma(gkb, mla_k_norm.to_broadcast([128, 96]), [], ["gkb"], "c12")

        ctr = [0]

        def uid():
            ctr[0] += 1
            return ctr[0]

        def rms_rows(src, Dw, gt, dst, sq, key, u=None):
            if u is None:
                u = uid() % 4
            ss = stat[:, 2 * u:2 * u + 1]
            rs = stat[:, 2 * u + 1:2 * u + 2]
            sn = "stat%d" % u
            act(lambda e: e.activation(out=sq, in_=src, func=ACT.Square, scale=1.0 / math.sqrt(Dw), accum_out=ss),
                key["r"], ["sq" + key["s"], sn])
            dve(lambda e: e.tensor_scalar(out=rs, in0=ss, scalar1=EPS, scalar2=None, op0=ALU.add), [sn], [sn + "a"])
            act(lambda e: e.activation(out=rs, in_=rs, func=ACT.Sqrt), [sn + "a"], [sn + "b"])
            dve(lambda e: e.reciprocal(out=rs, in_=rs), [sn + "b"], [sn + "c"])
            dve(lambda e: e.scalar_tensor_tensor(out=dst, in0=src, scalar=rs, in1=gt, op0=ALU.mult, op1=ALU.mult),
                key["r"] + [sn + "c"] + key["g"], key["w"])

        nrc = [0]

        def norm_rope(src3, H, Dh, gt, gkey, rd, cos_t, sin_t, dst3, wk, rkeys, wkeys, si=None):
            if si is None:
                si = nrc[0] % len(wk)
                nrc[0] += 1
            sq, qn, t1, t2, st_ = wk[si]
            R = lambda nm: "nr_%s%d" % (nm, si)
            sq3 = sq[:, 0:H * Dh].rearrange("p (h d) -> p h d", h=H)
            qn3 = qn[:, 0:H * Dh].rearrange("p (h d) -> p h d", h=H)
            ss = st_[:, 0:H]
            rs = st_[:, 8:8 + H]
            act(lambda e: e.activation(out=sq3, in_=src3, func=ACT.Square), rkeys, [R("sq")])
            dve(lambda e: e.tensor_reduce(out=ss, in_=sq3, axis=AX.X, op=ALU.add), [R("sq")], [R("ss")])
            dve(lambda e: e.tensor_scalar(out=rs, in0=ss, scalar1=1.0 / Dh, scalar2=EPS, op0=ALU.mult, op1=ALU.add),
                [R("ss")], [R("rs0")])
            act(lambda e: e.activation(out=rs, in_=rs, func=ACT.Sqrt), [R("rs0")], [R("rs1")])
            dve(lambda e: e.reciprocal(out=rs, in_=rs), [R("rs1")], [R("rs")])
            dve(lambda e: e.tensor_tensor(out=qn3, in0=src3, in1=bc(rs, 2, [128, H, Dh]), op=ALU.mult),
                rkeys + [R("rs")], [R("qn")])
            pool(lambda e: e.tensor_tensor(out=qn3, in0=qn3, in1=bc(gt, 1, [128, H, Dh]), op=ALU.mult),
                 [R("qn"), gkey], [R("qn")])
            nd = Dh - rd
            hf = rd // 2
            if nd > 0:
                act(lambda e: e.activation(out=dst3[:, :, 0:nd], in_=qn3[:, :, 0:nd], func=ACT.Copy), [R("qn")], wkeys)
            x1_ = qn3[:, :, nd:nd + hf]
            x2_ = qn3[:, :, nd + hf:Dh]
            cb = bc(cos_t, 1, [128, H, hf])
            sb_ = bc(sin_t, 1, [128, H, hf])
            t13 = t1[:, 0:H * hf].rearrange("p (h d) -> p h d", h=H)
            t23 = t2[:, 0:H * hf].rearrange("p (h d) -> p h d", h=H)
            t33 = t1[:, 256:256 + H * hf].rearrange("p (h d) -> p h d", h=H)
            t43 = t2[:, 256:256 + H * hf].rearrange("p (h d) -> p h d", h=H)
            dve(lambda e: e.tensor_tensor(out=t13, in0=x1_, in1=cb, op=ALU.mult), [R("qn")], [R("t1")])
            pool(lambda e: e.tensor_tensor(out=t23, in0=x2_, in1=sb_, op=ALU.mult), [R("qn")], [R("t2")])
            dve(lambda e: e.tensor_tensor(out=t33, in0=x2_, in1=cb, op=ALU.mult), [R("qn")], [R("t3")])
            pool(lambda e: e.tensor_tensor(out=t43, in0=x1_, in1=sb_, op=ALU.mult), [R("qn")], [R("t4")])
            dve(lambda e: e.tensor_tensor(out=dst3[:, :, nd:nd + hf], in0=t13, in1=t23, op=ALU.subtract),
                [R("t1"), R("t2")], wkeys)
            dve(lambda e: e.tensor_tensor(out=dst3[:, :, nd + hf:Dh], in0=t33, in1=t43, op=ALU.add),
                [R("t3"), R("t4")], wkeys)

        def load_w_cast(dst, src, key, rk):
            return dma_cast(dst, src, [], [rk], key)

        def norm_transpose(i, src, xT_dst, hb, sq, tpb, rsrc):
            rms_rows(src, D, g_bc, hb, sq, {"r": rsrc, "s": "", "g": ["g_bc"], "w": [("hb", i % 2)]})
            tp = pst(tpb)
            for kc in range(8):
                pe(lambda e, kc=kc: e.transpose(out=tp[:, kc, :], in_=hb[:, kc * 128:(kc + 1) * 128], identity=identb),
                   [("hb", i % 2), "identb"], [("ps", tpb)])
            act(lambda e: e.activation(out=xT_dst[:, :, i * 128:(i + 1) * 128], in_=tp, func=ACT.Copy),
                [("ps", tpb)], [("xT", i)])

        hnT = A.alloc([128, 8, S], BF16)
        yTa = A.alloc([128, 4, S], BF16)
        yTb = A.alloc([128, 4, S], BF16)
        mA = A.mark()
        xb = [A.alloc([128, D], F32) for _ in range(2)]
        hbs = [A.alloc([128, D], BF16) for _ in range(2)]
        sqs = A.alloc([128, D], F32)
        for i in range(NT):
            dma(xb[i % 2], x[i * 128:(i + 1) * 128, :], [], [("xb", i % 2)], ("xb", i % 2))
            norm_transpose(i, xb[i % 2], hnT, hbs[i % 2], sqs, i % 2, [("xb", i % 2)])
        P.barrier()
        A.reset(mA)

        qT = A.alloc([128, 4, S], BF16)
        kT = A.alloc([128, 4, S], BF16)
        vaug = A.alloc([128, NT, 4, 65], BF16)
        wbuf = A.alloc([128, 8, 1024], BF16)
        qs = [A.alloc([128, 4, 96], BF16) for _ in range(2)]
        ks = [A.alloc([128, 4, 96], BF16) for _ in range(2)]
        wk = [tuple([A.alloc([128, 512], F32) for _ in range(4)] + [A.alloc([128, 16], F32)]) for _ in range(4)]
        kmT = A.alloc([128, 4, 8], BF16)
        kmF = A.alloc([128, 4, 8], F32)
        gsb2 = [A.alloc([128, 4, 8], F32) for _ in range(2)]
        cmpb2 = [A.alloc([128, 4, 8, 8], F32) for _ in range(2)]
        rank2 = [A.alloc([128, 4, 8], F32) for _ in range(2)]
        pbuf = [A.alloc([128, 512], BF16) for _ in range(2)]
        ytok = [A.alloc([128, 4, 256], BF16) for _ in range(2)]
        rec = A.alloc([128, 8], F32)
        latb2 = [A.alloc([128, 384], BF16) for _ in range(2)]
        latT2 = [A.alloc([128, 3, 128], BF16) for _ in range(2)]
        kcat2 = [A.alloc([128, 4, 96], F32) for _ in range(2)]
        kpes2 = [A.alloc([128, 32], F32) for _ in range(2)]
        sql2 = [A.alloc([128, 384], F32) for _ in range(2)]

        pool(lambda e: e.memset(vaug[:, :, :, 64:65], 1.0), [], ["vaug_ones"])
        pool(lambda e: e.memset(qT, 0.0), [], ["qT"])
        pool(lambda e: e.memset(kT, 0.0), [], ["kT"])

        def attention(half, Kd, scale, yT):
            it = 0
            for Q in range(NQ):
                yt = ytok[Q % 2]
                for hh in range(4):
                    accb = 4 + (it % 2)
                    acc3 = psb(accb).rearrange("p (t c) -> p t c", t=4)
                    nj = 4 * Q + 4

                    def s_stage(j, Q=Q, hh=hh):
                        tlo = max(j, 4 * Q)
                        ncols = (4 * Q + 4 - tlo) * 128
                        qc0 = tlo * 128
                        sbk = 6 + (j % 2)
                        sps = psb(sbk)
                        diag = j >= 4 * Q
                        pe(lambda e: e.matmul(sps[:, 0:ncols], lhsT=kT[0:Kd, hh, j * 128:(j + 1) * 128],
                                              rhs=qT[0:Kd, hh, qc0:qc0 + ncols], start=True, stop=not diag),
                           ["qT", "kT"], [("ps", sbk)])
                        if diag:
                            pe(lambda e: e.matmul(sps[:, 0:128], lhsT=identb, rhs=trib, start=False, stop=True),
                               ["identb", "trib"], [("ps", sbk)])
                        pb = pbuf[j % 2]
                        act(lambda e: e.activation(out=pb[:, 0:ncols], in_=sps[:, 0:ncols], func=ACT.Exp, scale=scale),
                            [("ps", sbk)], [("pb", j % 2)])

                    def pv_stage(j, Q=Q, hh=hh, acc3=acc3, accb=accb):
                        tlo = max(j, 4 * Q)
                        pb = pbuf[j % 2]
                        for t in range(tlo, 4 * Q + 4):
                            c = (t - tlo) * 128
                            first = (j == 0 and t == tlo)
                            pe(lambda e, c=c, t=t, first=first: e.matmul(
                                acc3[:, t - 4 * Q, 0:65], lhsT=pb[:, c:c + 128], rhs=vaug[:, j, hh, :],
                                start=first, stop=(j == t), skip_group_check=True),
                               [("pb", j % 2), "vaug", "vaug_ones"], [("ps", accb)])

                    s_stage(0)
                    for j in range(nj):
                        if j + 1 < nj:
                            s_stage(j + 1)
                        pv_stage(j)
                    dve(lambda e, acc3=acc3: e.reciprocal(out=rec[:, 0:4], in_=acc3[:, :, 64]), [("ps", accb)], ["rec"])
                    dve(lambda e, acc3=acc3, yt=yt, hh=hh: e.tensor_tensor(
                        out=yt[:, :, hh * 64:(hh + 1) * 64], in0=acc3[:, :, 0:64],
                        in1=bc(rec[:, 0:4], 2, [128, 4, 64]), op=ALU.mult),
                        [("ps", accb), "rec"], [("yt", Q % 2)])
                    it += 1
                tpb = Q % 2
                tp = pst(tpb)
                for fc in range(2):
                    for tl in range(4):
                        pe(lambda e, fc=fc, tl=tl, yt=yt, tp=tp: e.transpose(
                            out=tp[:, fc * 4 + tl, :], in_=yt[:, tl, fc * 128:(fc + 1) * 128], identity=identb),
                           [("yt", Q % 2), "identb"], [("ps", tpb)])
                act(lambda e, tp=tp, Q=Q: e.activation(
                    out=yT[:, half * 2:half * 2 + 2, Q * 512:(Q + 1) * 512],
                    in_=tp.rearrange("p (f t) c -> p f (t c)", f=2), func=ACT.Copy),
                    [("ps", tpb)], ["yT"])

        def load_moba(half_):
            for part in range(3):
                c0 = part * 512 + half_ * 4 * 64
                load_w_cast(wbuf[:, :, part * 256:(part + 1) * 256],
                            w_in[:, c0:c0 + 256].rearrange("(kc p) n -> p kc n", p=128), ("wb", part), "wbuf")

        def load_mla(half_):
            h0_ = half_ * 4
            load_w_cast(wbuf[:, :, 0:416], w_in[:, 1536:1952].rearrange("(kc p) n -> p kc n", p=128), ("wb", 0), "wbuf")
            load_w_cast(wbuf[:, 0:2, 416:800], w_uq[:, h0_ * 96:(h0_ + 4) * 96].rearrange("(kc p) n -> p kc n", p=128),
                        ("wb", 1), "wbuf")
            load_w_cast(wbuf[:, 2, 416:928], w_ukv[:, h0_ * 128:(h0_ + 4) * 128], ("wb", 2), "wbuf")

        load_moba(0)
        for half in range(2):
            h0 = half * 4
            pool(lambda e: e.memset(qT[64:72, :, :], 0.0), [], ["qT"])
            for b in range(2):
                pool(lambda e, b=b: e.memset(qs[b][:, :, 64:72], 0.0), [], [("qs", b)])
            def k_tile(i):
                c = i // 2
                mb = i % 2
                pk = psb(mb)
                pv = psb(2 + mb)
                for kc in range(8):
                    pe(lambda e, kc=kc, i=i, pk=pk: e.matmul(pk[:, 0:256], lhsT=hnT[:, kc, i * 128:(i + 1) * 128],
                                                              rhs=wbuf[:, kc, 256:512], start=(kc == 0), stop=(kc == 7)),
                       ["wbuf"], [("ps", mb)])
                for kc in range(8):
                    pe(lambda e, kc=kc, i=i, pv=pv: e.matmul(pv[:, 0:256], lhsT=hnT[:, kc, i * 128:(i + 1) * 128],
                                                              rhs=wbuf[:, kc, 512:768], start=(kc == 0), stop=(kc == 7)),
                       ["wbuf"], [("ps", 2 + mb)])
                act(lambda e, i=i, pv=pv: e.activation(out=vaug[:, i, :, 0:64],
                                                        in_=pv[:, 0:256].rearrange("p (h d) -> p h d", h=4), func=ACT.Copy),
                    [("ps", 2 + mb)], ["vaug"])
                ksb = ks[i % 2]
                norm_rope(pk[:, 0:256].rearrange("p (h d) -> p h d", h=4), 4, 64, gka, "gka", 64,
                          cosA[:, i, :], sinA[:, i, :], ksb[:, :, 0:64], wk, [("ps", mb), "cosA", "sinA"], [("ks", i % 2)],
                          si=(i % 2) * 2)
                if c > 0:
                    pool(lambda e, ksb=ksb, c=c: e.memset(ksb[:, :, 64:64 + c], 0.0), [], [("ks", i % 2)])
                pool(lambda e, ksb=ksb, c=c: e.memset(ksb[:, :, 64 + c:65 + c], 1.0), [], [("ks", i % 2)])
                if c < 7:
                    pool(lambda e, ksb=ksb, c=c: e.memset(ksb[:, :, 65 + c:72], 0.0), [], [("ks", i % 2)])
                tpb = 4 + (i % 2)
                tp = pst(tpb)
                for hh in range(4):
                    pe(lambda e, hh=hh, ksb=ksb, tp=tp: e.transpose(out=tp[0:72, hh, :], in_=ksb[:, hh, 0:72], identity=identb),
                       [("ks", i % 2), "identb"], [("ps", tpb)])
                act(lambda e, i=i, tp=tp: e.activation(out=kT[0:72, :, i * 128:(i + 1) * 128], in_=tp[0:72, 0:4, :], func=ACT.Copy),
                    [("ps", tpb)], ["kT"])
            P.interleave(k_tile, range(NT))
            dve(lambda e: e.tensor_reduce(out=kmF[0:64, :, 0:NB],
                                          in_=kT[0:64, :, :].rearrange("p h (n k) -> p h n k", k=256),
                                          axis=AX.X, op=ALU.add), ["kT"], ["kmF"])
            dve(lambda e: e.tensor_scalar(out=kmT[0:64, :, 0:NB], in0=kmF[0:64, :, 0:NB], scalar1=1.0 / 256.0, scalar2=None,
                                          op0=ALU.mult), ["kmF"], ["kmT"])
            def q_tile(i):
                c = i // 2
                mb = i % 2
                gsb, cmpb, rank = gsb2[mb], cmpb2[mb], rank2[mb]
                G = lambda nm, mb=mb: "%s%d" % (nm, mb)
                pq = psb(mb)
                for kc in range(8):
                    pe(lambda e, kc=kc, i=i, pq=pq: e.matmul(pq[:, 0:256], lhsT=hnT[:, kc, i * 128:(i + 1) * 128],
                                                              rhs=wbuf[:, kc, 0:256], start=(kc == 0), stop=(kc == 7)),
                       ["wbuf"], [("ps", mb)])
                qsb = qs[i % 2]
                norm_rope(pq[:, 0:256].rearrange("p (h d) -> p h d", h=4), 4, 64, gqa, "gqa", 64,
                          cosA[:, i, :], sinA[:, i, :], qsb[:, :, 0:64], wk, [("ps", mb), "cosA", "sinA"], [("qs", i % 2)],
                          si=(i % 2) * 2)
                tpb = 4 + (i % 2)
                tp = pst(tpb)
                for hh in range(4):
                    pe(lambda e, hh=hh, qsb=qsb, tp=tp: e.transpose(out=tp[0:64, hh, :], in_=qsb[:, hh, 0:64], identity=identb),
                       [("qs", i % 2), "identb"], [("ps", tpb)])
                act(lambda e, i=i, tp=tp: e.activation(out=qT[0:64, :, i * 128:(i + 1) * 128], in_=tp[0:64, 0:4, :], func=ACT.Copy),
                    [("ps", tpb)], ["qT", ("qTi", i)])
                if c > 3:
                    gb = 2 + mb
                    gps = psb(gb)[:, 0:32].rearrange("p (h n) -> p h n", h=4)
                    for hh in range(4):
                        pe(lambda e, hh=hh, i=i, gps=gps, c=c: e.matmul(
                            gps[:, hh, 0:c], lhsT=qT[0:64, hh, i * 128:(i + 1) * 128], rhs=kmT[0:64, hh, 0:c],
                            start=(hh == 0), stop=(hh == 3), skip_group_check=True),
                           [("qTi", i), "kmT"], [("ps", gb)])
                    act(lambda e, gps=gps, c=c, gsb=gsb: e.activation(out=gsb[:, :, 0:c], in_=gps[:, :, 0:c], func=ACT.Copy),
                        [("ps", gb)], [G("gsb")])
                    gv = gsb[:, :, 0:c]
                    dve(lambda e, gv=gv, c=c, cmpb=cmpb: e.tensor_tensor(out=cmpb[:, :, 0:c, 0:c], in0=bc(gv, 2, [128, 4, c, c]),
                                                                        in1=bc(gv, 3, [128, 4, c, c]), op=ALU.is_gt),
                        [G("gsb")], [G("cmpb")])
                    dve(lambda e, c=c, cmpb=cmpb, rank=rank: e.tensor_reduce(out=rank[:, :, 0:c], in_=cmpb[:, :, 0:c, 0:c], axis=AX.X, op=ALU.add),
                        [G("cmpb")], [G("rank")])
                    dve(lambda e, c=c, qsb=qsb, rank=rank: e.tensor_scalar(out=qsb[:, :, 64:64 + c], in0=rank[:, :, 0:c], scalar1=2.5,
                                                                          scalar2=NEG, op0=ALU.is_gt, op1=ALU.mult),
                        [G("rank")], [("qs", i % 2)])
                    tpb2 = 6 + (i % 2)
                    tp2 = pst(tpb2)
                    for hh in range(4):
                        pe(lambda e, hh=hh, qsb=qsb, tp2=tp2: e.transpose(out=tp2[0:72, hh, :], in_=qsb[:, hh, 0:72], identity=identb),
                           [("qs", i % 2), "identb"], [("ps", tpb2)])
                    act(lambda e, i=i, tp2=tp2: e.activation(out=qT[64:72, :, i * 128:(i + 1) * 128], in_=tp2[64:72, 0:4, :],
                                                              func=ACT.Copy),
                        [("ps", tpb2)], ["qT", ("qTi", i)])
            P.interleave(q_tile, range(NT))
            if half == 0:
                load_moba(1)
            else:
                load_mla(0)
            attention(half, 128, 64 ** -0.5, yTa)

        for half in range(2):
            h0 = half * 4
            def mla_tile(i):
                mb = i % 2
                latb, latT, kcat, kpes, sql = latb2[mb], latT2[mb], kcat2[mb], kpes2[mb], sql2[mb]
                L = lambda nm, mb=mb: "%s%d" % (nm, mb)
                pl = psb(mb)
                for kc in range(8):
                    pe(lambda e, kc=kc, i=i, pl=pl: e.matmul(pl[:, 0:416], lhsT=hnT[:, kc, i * 128:(i + 1) * 128],
                                                              rhs=wbuf[:, kc, 0:416], start=(kc == 0), stop=(kc == 7)),
                       ["wbuf"], [("ps", mb)])
                rms_rows(pl[:, 0:256], 256, gql, latb[:, 0:256], sql[:, 0:256],
                         {"r": [("ps", mb)], "s": L("l"), "g": ["gql"], "w": [L("latb")]}, u=mb * 2)
                rms_rows(pl[:, 256:384], 128, gkvl, latb[:, 256:384], sql[:, 256:384],
                         {"r": [("ps", mb)], "s": L("l"), "g": ["gkvl"], "w": [L("latb")]}, u=mb * 2 + 1)
                act(lambda e, pl=pl, kpes=kpes: e.activation(out=kpes, in_=pl[:, 384:416], func=ACT.Copy), [("ps", mb)], [L("kpes")])
                tpb = 4 + (i % 2)
                tp = pst(tpb)
                for cc in range(3):
                    pe(lambda e, cc=cc, tp=tp, latb=latb: e.transpose(out=tp[:, cc, :], in_=latb[:, cc * 128:(cc + 1) * 128], identity=identb),
                       [L("latb"), "identb"], [("ps", tpb)])
                act(lambda e, tp=tp, latT=latT: e.activation(out=latT, in_=tp[:, 0:3, :], func=ACT.Copy), [("ps", tpb)], [L("latT")])
                pqb = psb(2 + mb)
                for cc in range(2):
                    pe(lambda e, cc=cc, pqb=pqb, latT=latT: e.matmul(pqb[:, 0:384], lhsT=latT[:, cc, :], rhs=wbuf[:, cc, 416:800],
                                                                      start=(cc == 0), stop=(cc == 1)),
                       [L("latT"), "wbuf"], [("ps", 2 + mb)])
                qsb = qs[i % 2]
                norm_rope(pqb[:, 0:384].rearrange("p (h d) -> p h d", h=4), 4, 96, gqb, "gqb", 32,
                          cosB[:, i, :], sinB[:, i, :], qsb, wk, [("ps", 2 + mb), "cosB", "sinB"], [("qs", i % 2)], si=mb * 2)
                pkv = psb(mb)
                pe(lambda e, pkv=pkv, latT=latT: e.matmul(pkv[:, 0:512], lhsT=latT[:, 2, :], rhs=wbuf[:, 2, 416:928], start=True, stop=True),
                   [L("latT"), "wbuf", L("kpes")], [("ps", mb)])
                pkv3 = pkv.rearrange("p (h d) -> p h d", h=4)
                act(lambda e, i=i, pkv3=pkv3: e.activation(out=vaug[:, i, :, 0:64], in_=pkv3[:, :, 64:128], func=ACT.Copy),
                    [("ps", mb)], ["vaug"])
                act(lambda e, pkv3=pkv3, kcat=kcat: e.activation(out=kcat[:, :, 0:64], in_=pkv3[:, :, 0:64], func=ACT.Copy),
                    [("ps", mb)], [L("kcat")])
                dve(lambda e, kcat=kcat, kpes=kpes: e.tensor_copy(out=kcat[:, :, 64:96], in_=bc(kpes, 1, [128, 4, 32])),
                    [L("kpes")], [L("kcat")])
                ksb = ks[i % 2]
                norm_rope(kcat, 4, 96, gkb, "gkb", 32, cosB[:, i, :], sinB[:, i, :], ksb, wk,
                          [L("kcat"), "cosB", "sinB"], [("ks", i % 2)], si=mb * 2 + 1)
                tpb2 = 6 + (i % 2)
                tp2 = pst(tpb2)
                for hh in range(4):
                    pe(lambda e, hh=hh, qsb=qsb, tp2=tp2: e.transpose(out=tp2[0:96, hh, :], in_=qsb[:, hh, :], identity=identb),
                       [("qs", i % 2), "identb"], [("ps", tpb2)])
                    pe(lambda e, hh=hh, ksb=ksb, tp2=tp2: e.transpose(out=tp2[0:96, 4 + hh, :], in_=ksb[:, hh, :], identity=identb),
                       [("ks", i % 2), "identb"], [("ps", tpb2)])
                act(lambda e, i=i, tp2=tp2: e.activation(out=qT[0:96, :, i * 128:(i + 1) * 128], in_=tp2[0:96, 0:4, :], func=ACT.Copy),
                    [("ps", tpb2)], ["qT"])
                act(lambda e, i=i, tp2=tp2: e.activation(out=kT[0:96, :, i * 128:(i + 1) * 128], in_=tp2[0:96, 4:8, :], func=ACT.Copy),
                    [("ps", tpb2)], ["kT"])
            P.interleave(mla_tile, range(NT))
            if half == 0:
                load_mla(1)
            attention(half, 128, 96 ** -0.5, yTb)

        if debug:
            for nm, yT in (("ya", yTa), ("yb", yTb)):
                P.barrier()
                for c4 in range(4):
                    for Q in range(NQ):
                        dbf = wk[0][0]
                        dve(lambda e, yT=yT, c4=c4, Q=Q, dbf=dbf: e.tensor_copy(out=dbf, in_=yT[:, c4, Q * 512:(Q + 1) * 512]),
                            [], ["dbf"])
                        dma(dbg[nm][c4 * 128:(c4 + 1) * 128, Q * 512:(Q + 1) * 512], dbf, ["dbf"], [], "dbg", final=True)
        P.barrier()
        A.reset(mA)

        mgT = A.alloc([128, 8, S], BF16)
        wg = [A.alloc([128, 8, 256], BF16) for _ in range(2)]
        wab = [A.alloc([128, 4, 256], BF16) for _ in range(2)]
        sg = [A.alloc([128, 512], F32) for _ in range(2)]
        m1 = [A.alloc([128, 512], F32) for _ in range(2)]
        it = 0
        def load_c(fc):
            w2 = fc % 2
            load_w_cast(wg[w2][:, :, 0:128], w_in[:, 1952 + fc * 128:1952 + (fc + 1) * 128].rearrange("(kc p) n -> p kc n", p=128),
                        ("wg", w2, 0), ("wg", w2))
            load_w_cast(wg[w2][:, :, 128:256], w_in[:, 2976 + fc * 128:2976 + (fc + 1) * 128].rearrange("(kc p) n -> p kc n", p=128),
                        ("wg", w2, 1), ("wg", w2))
            load_w_cast(wab[w2][:, :, 0:128], w_branch_a[:, fc * 128:(fc + 1) * 128].rearrange("(kc p) n -> p kc n", p=128),
                        ("wab", w2, 0), ("wab", w2))
            load_w_cast(wab[w2][:, :, 128:256], w_branch_b[:, fc * 128:(fc + 1) * 128].rearrange("(kc p) n -> p kc n", p=128),
                        ("wab", w2, 1), ("wab", w2))

        load_c(0)
        for fc in range(8):
            w2 = fc % 2
            if fc + 1 < 8:
                load_c(fc + 1)
            for tc in range(NQ):
                b0 = (it % 2) * 4
                cols = slice(tc * 512, (tc + 1) * 512)
                for br in range(2):
                    pg_ = psb(b0 + br * 2)
                    py_ = psb(b0 + br * 2 + 1)
                    yT = yTa if br == 0 else yTb
                    for kc in range(8):
                        pe(lambda e, kc=kc, pg_=pg_, br=br, w2=w2, cols=cols: e.matmul(
                            pg_, lhsT=wg[w2][:, kc, br * 128:(br + 1) * 128], rhs=hnT[:, kc, cols],
                            start=(kc == 0), stop=(kc == 7)), [("wg", w2)], [("ps", b0 + br * 2)])
                    for kc in range(4):
                        pe(lambda e, kc=kc, py_=py_, br=br, w2=w2, cols=cols, yT=yT: e.matmul(
                            py_, lhsT=wab[w2][:, kc, br * 128:(br + 1) * 128], rhs=yT[:, kc, cols],
                            start=(kc == 0), stop=(kc == 3)), [("wab", w2)], [("ps", b0 + br * 2 + 1)])
                    act(lambda e, pg_=pg_, br=br: e.activation(out=sg[br], in_=pg_, func=ACT.Sigmoid),
                        [("ps", b0 + br * 2)], [("sg", br)])
                    dve(lambda e, py_=py_, br=br: e.tensor_tensor(out=m1[br], in0=py_, in1=sg[br], op=ALU.mult),
                        [("ps", b0 + br * 2 + 1), ("sg", br)], [("m1", br)])
                pool(lambda e, fc=fc, cols=cols: e.tensor_tensor(out=mgT[:, fc, cols], in0=m1[0], in1=m1[1], op=ALU.add),
                     [("m1", 0), ("m1", 1)], ["mgT"])
                it += 1
        P.barrier()
        A.reset(m0)
        x1 = A.alloc([128, NT, D], F32)
        mD = A.mark()
        assert mD <= mA
        A.reset(mA)
        mgT2 = A.alloc([128, 8, S], BF16)
        wo = A.alloc([128, 8, D], BF16)
        for hlf in range(2):
            load_w_cast(wo[:, hlf * 4:(hlf + 1) * 4, :], w_out[hlf * 512:(hlf + 1) * 512, :].rearrange("(kc p) n -> p kc n", p=128),
                        ("wo", hlf), "wo")
        for i in range(NT):
            dma(x1[:, i, :], x[i * 128:(i + 1) * 128, :], [], [("x1", i)], ("x1l", i % 4))
            for nh in range(2):
                pb_ = (i * 2 + nh) % 8
                pp = psb(pb_)
                for kc in range(8):
                    pe(lambda e, kc=kc, i=i, nh=nh, pp=pp: e.matmul(pp, lhsT=mgT2[:, kc, i * 128:(i + 1) * 128],
                                                                    rhs=wo[:, kc, nh * 512:(nh + 1) * 512],
                                                                    start=(kc == 0), stop=(kc == 7)),
                       ["wo"], [("ps", pb_)])
                dve(lambda e, i=i, nh=nh, pp=pp: e.tensor_tensor(out=x1[:, i, nh * 512:(nh + 1) * 512], in0=pp,
                                                                 in1=x1[:, i, nh * 512:(nh + 1) * 512], op=ALU.add),
                    [("ps", pb_), ("x1", i)], [("x1", i)])
            if debug:
                dma(dbg["x1"][i * 128:(i + 1) * 128, :], x1[:, i, :], [("x1", i)], [], "dbg", final=True)
        P.barrier()
        A.reset(mD)

        NG = NT // 4
        hnk = A.alloc([128, NT, D], BF16)
        Wr = A.alloc([128, NT, NEXP], F32)
        posm = A.alloc([128, NT, NEXP], F32)
        bguT = A.alloc([128, 16, NEXP], F32)
        iot = A.alloc([128, 128], F32)
        mD2 = A.mark()
        bdn = A.alloc([128, D], F32)
        wr = A.alloc([128, 8, NEXP], F32)
        brb = A.alloc([128, NEXP], F32)
        maskb = A.alloc([128, NT, NEXP], BF16)
        triu = A.alloc([128, 128], BF16)
        onesb = A.alloc([128, 128], BF16)
        hf = A.alloc([128, D], F32)
        hfT = A.alloc([128, 8, 128], F32)
        sqs = A.alloc([128, D], F32)
        lgs = A.alloc([128, NEXP], F32)
        em = A.alloc([128, NEXP], F32)
        mk = A.alloc([128, NEXP], F32)
        m8 = A.alloc([128, 8], F32)
        bgl = A.alloc([128, 2048], F32)
        WrT = A.alloc([128, 128], F32)

        dma(g_bc, g_ffn.to_broadcast([128, D]), [], ["g_bc"], "c6")
        dma(wr, w_router.rearrange("(kc p) n -> p kc n", p=128), [], ["wr"], "c7")
        dma(brb, b_router.to_broadcast([128, NEXP]), [], ["brb"], "c8")
        dma(bgl[0:NEXP, :], b_gate_up, [], ["bgl"], "c9")
        dma(bdn[0:NEXP, :], b_down, [], ["bdn"], "c10")
        dma(iot, c_iota.to_broadcast([128, 128]), [], ["iot"], "c11")
        dma(tmpf, c_triu, [], ["tmpf"], "c12")
        dve(lambda e: e.tensor_copy(out=triu, in_=tmpf), ["tmpf"], ["triu"])
        pool(lambda e: e.memset(onesb, 1.0), [], ["onesb"])
        pbg = PS[:, 0:2, :].rearrange("p b n -> p (b n)")[:, 0:16 * NEXP].rearrange("p (j e) -> p j e", j=16)
        for j in range(16):
            pe(lambda e, j=j: e.transpose(out=pbg[:, j, :], in_=bgl[0:NEXP, j * 128:(j + 1) * 128], identity=identf[0:NEXP, 0:NEXP]),
               ["bgl", "identf"], [("ps", 0), ("ps", 1)])
        act(lambda e: e.activation(out=bguT, in_=pbg, func=ACT.Copy), [("ps", 0), ("ps", 1)], ["bguT"])

        hf2 = [hf, A.alloc([128, D], F32)]
        hfT2 = [hfT, A.alloc([128, 8, 128], F32)]
        lgs2 = [lgs, A.alloc([128, NEXP], F32)]
        em2 = [em, A.alloc([128, NEXP], F32)]
        mk2 = [mk, A.alloc([128, NEXP], F32)]
        m82 = [m8, A.alloc([128, 8], F32)]
        WrT2 = [WrT, A.alloc([128, 128], F32)]
        st2 = [stat, A.alloc([128, 64], F32)]

        def route_tile(i, sqs=sqs):
            p2 = i % 2
            hf, hfT, lgs, em, mk, m8, WrT, st = hf2[p2], hfT2[p2], lgs2[p2], em2[p2], mk2[p2], m82[p2], WrT2[p2], st2[p2]
            N = lambda nm, p2=p2: "%s_%d" % (nm, p2)
            B0 = 4 * p2
            src = x1[:, i, :]
            ss = st[:, 0:1]
            rs = st[:, 1:2]
            act(lambda e: e.activation(out=sqs, in_=src, func=ACT.Square, scale=1.0 / 32.0, accum_out=ss),
                [("x1", i)], ["sqs", N("stat")])
            dve(lambda e: e.tensor_scalar(out=rs, in0=ss, scalar1=EPS, scalar2=None, op0=ALU.add), [N("stat")], [N("stat2a")])
            act(lambda e: e.activation(out=rs, in_=rs, func=ACT.Sqrt), [N("stat2a")], [N("stat2b")])
            dve(lambda e: e.reciprocal(out=rs, in_=rs), [N("stat2b")], [N("stat2")])
            dve(lambda e: e.scalar_tensor_tensor(out=hf, in0=src, scalar=rs, in1=g_bc, op0=ALU.mult, op1=ALU.mult),
                [("x1", i), N("stat2"), "g_bc"], [N("hf")])
            act(lambda e: e.activation(out=hnk[:, i, :], in_=hf, func=ACT.Copy), [N("hf")], [("hnk", i)])
            pft = PS[:, B0:B0 + 2, :].rearrange("p b n -> p (b n)").rearrange("p (k c) -> p k c", k=8)
            for kc in range(8):
                pe(lambda e, kc=kc: e.transpose(out=pft[:, kc, :], in_=hf[:, kc * 128:(kc + 1) * 128], identity=identf),
                   [N("hf"), "identf"], [("ps", B0), ("ps", B0 + 1)])
            dve(lambda e: e.tensor_copy(out=hfT, in_=pft), [("ps", B0), ("ps", B0 + 1)], [N("hfT")])
            plg = psb(B0 + 2)
            for kc in range(8):
                pe(lambda e, kc=kc: e.matmul(plg[:, 0:NEXP], lhsT=hfT[:, kc, :], rhs=wr[:, kc, :],
                                             start=(kc == 0), stop=(kc == 7)),
                   [N("hfT"), "wr"], [("ps", B0 + 2)])
            dve(lambda e: e.tensor_tensor(out=lgs, in0=plg[:, 0:NEXP], in1=brb, op=ALU.add),
                [("ps", B0 + 2), "brb"], [N("lgs")])
            dve(lambda e: e.max(out=m8, in_=lgs), [N("lgs")], [N("m8")])
            dve(lambda e: e.tensor_scalar(out=mk, in0=lgs, scalar1=m8[:, 3:4], scalar2=None, op0=ALU.is_ge),
                [N("lgs"), N("m8")], [N("mk")])
            act(lambda e: e.activation(out=maskb[:, i, :], in_=mk, func=ACT.Copy), [N("mk")], [("maskb", i)])
            dve(lambda e: e.tensor_scalar(out=st[:, 2:3], in0=m8[:, 0:1], scalar1=-1.0, scalar2=None, op0=ALU.mult),
                [N("m8")], [N("negmax")])
            act(lambda e: e.activation(out=em, in_=lgs, func=ACT.Exp, bias=st[:, 2:3], scale=1.0),
                [N("lgs"), N("negmax")], [N("em")])
            dve(lambda e: e.tensor_tensor(out=em, in0=em, in1=mk, op=ALU.mult), [N("em"), N("mk")], [N("em2")])
            dve(lambda e: e.tensor_reduce(out=st[:, 3:4], in_=em, axis=AX.X, op=ALU.add), [N("em2")], [N("den")])
            dve(lambda e: e.reciprocal(out=st[:, 4:5], in_=st[:, 3:4]), [N("den")], [N("rden")])
            dve(lambda e: e.tensor_scalar(out=Wr[:, i, :], in0=em, scalar1=st[:, 4:5], scalar2=None, op0=ALU.mult),
                [N("em2"), N("rden")], [("Wr", i)])
            ppos = psb(B0 + 2)[:, 64:64 + NEXP]
            i0 = (i // 4) * 4
            for i2 in range(i0, i + 1):
                lhs = triu if i2 == i else onesb
                pe(lambda e, i2=i2, lhs=lhs: e.matmul(ppos, lhsT=lhs, rhs=maskb[:, i2, :], start=(i2 == i0), stop=(i2 == i)),
                   [("maskb", i2), "triu", "onesb"], [("ps", B0 + 2)])
            dve(lambda e: e.scalar_tensor_tensor(out=st[:, 32:64], in0=ppos, scalar=1.0, in1=mk,
                                                 op0=ALU.add, op1=ALU.mult), [("ps", B0 + 2), N("mk")], [N("posp")])
            dve(lambda e: e.tensor_scalar(out=posm[:, i, :], in0=st[:, 32:64], scalar1=-1.0, scalar2=None, op0=ALU.add),
                [N("posp")], [("posm", i)])
            pwt = psb(B0 + 3)
            pe(lambda e: e.transpose(out=pwt[0:NEXP, 0:128], in_=Wr[:, i, :], identity=identf),
               [("Wr", i), "identf"], [("ps", B0 + 3)])
            act(lambda e: e.activation(out=WrT[0:NEXP, :], in_=pwt[0:NEXP, 0:128], func=ACT.Copy), [("ps", B0 + 3)], [N("WrT")])
            for nh in range(2):
                pbb = psb(B0 + nh)
                pe(lambda e, nh=nh, pbb=pbb: e.matmul(pbb, lhsT=WrT[0:NEXP, :], rhs=bdn[0:NEXP, nh * 512:(nh + 1) * 512],
                                                       start=True, stop=True), [N("WrT"), "bdn"], [("ps", B0 + nh)])
                dve(lambda e, nh=nh, pbb=pbb: e.tensor_tensor(out=x1[:, i, nh * 512:(nh + 1) * 512], in0=pbb,
                                                               in1=x1[:, i, nh * 512:(nh + 1) * 512], op=ALU.add),
                    [("ps", B0 + nh), ("x1", i)], [("x1", i)])

        P.interleave(route_tile, range(NT))
        P.barrier()
        A.reset(mD2)
        SL = NG * 128
        wgu = [A.alloc([128, 8, 2, 128], BF16) for _ in range(4)]
        wdn = [A.alloc([128, 8, 512], BF16) for _ in range(2)]
        sel = [A.alloc([128, NT, 128], BF16) for _ in range(2)]
        XeT = A.alloc([128, 8, SL], BF16)
        hT = A.alloc([128, 8, SL], BF16)
        Yb = A.alloc([128, NG, D], BF16)
        gs_ = [A.alloc([128, SL], F32) for _ in range(2)]
        us_ = [A.alloc([128, SL], F32) for _ in range(2)]
        sgm = [A.alloc([128, SL], F32) for _ in range(2)]
        selw = A.alloc([128, NT, 128], BF16)
        selwT = A.alloc([128, NT, 128], BF16)
        ctmp = [A.alloc([128, 512], F32) for _ in range(2)]
        issued = [0]
        PF = 3

        def issue_gu(upto):
            while issued[0] < min(upto, n_exp * 8):
                n = issued[0]
                ex_, j_ = divmod(n, 8)
                t_ = wgu[n % 4]
                k_ = ("wgu", n % 4)
                for u in range(2):
                    wsrc = w_gate_up[ex_][:, u * 1024 + j_ * 128:u * 1024 + (j_ + 1) * 128].rearrange("(kc p) n -> p kc n", p=128)
                    load_w_cast(t_[:, :, u, :], wsrc, ("wgu", n % 4, u), k_)
                issued[0] += 1

        def make_sel(ex_):
            sl_ = sel[ex_ % 2]
            for i in range(NT):
                dve(lambda e, i=i, sl_=sl_, ex_=ex_: e.tensor_scalar(out=sl_[:, i, :], in0=iot, scalar1=posm[:, i, ex_:ex_ + 1],
                                                                    scalar2=None, op0=ALU.is_equal),
                    ["iot", ("posm", i)], [("sel", ex_ % 2)])

        cit = [0]
        itc = [0]
        npd_ = [0]

        def gather_pass(ex, ps_):
            sl = sel[ex % 2]
            for f4 in range(4):
                f = ps_ * 4 + f4
                pf_ = psb(f4)
                for g in range(NG):
                    for i in range(g * 4, g * 4 + 4):
                        pe(lambda e, i=i, f=f, g=g, pf_=pf_, sl=sl: e.matmul(
                            pf_[:, g * 128:(g + 1) * 128], lhsT=hnk[:, i, f * 128:(f + 1) * 128], rhs=sl[:, i, :],
                            start=(i == g * 4), stop=(i == g * 4 + 3), skip_group_check=True),
                           [("sel", ex % 2)], [("ps", f4)])
            act(lambda e, ps_=ps_: e.activation(out=XeT[:, ps_ * 4:(ps_ + 1) * 4, :], in_=PS[:, 0:4, 0:SL], func=ACT.Copy),
                [("ps", 0), ("ps", 1), ("ps", 2), ("ps", 3)], ["XeT"])

        def gateup(ex):
            issue_gu(ex * 8 + PF)
            dn_tiles = []
            for nh in range(2):
                t = wdn[npd_[0] % 2]
                k = ("wdn", npd_[0] % 2)
                load_w_cast(t, w_down[ex][:, nh * 512:(nh + 1) * 512].rearrange("(kc p) n -> p kc n", p=128), k, k)
                dn_tiles.append((t, k))
                npd_[0] += 1
            for j in range(8):
                n = ex * 8 + j
                issue_gu(n + PF)
                t = wgu[n % 4]
                k = ("wgu", n % 4)
                it = itc[0]
                b0 = 4 + (it % 2) * 2
                pg_ = psb(b0)[:, 0:SL]
                pu_ = psb(b0 + 1)[:, 0:SL]
                for kc in range(8):
                    pe(lambda e, kc=kc, t=t, pg_=pg_: e.matmul(pg_, lhsT=t[:, kc, 0, :], rhs=XeT[:, kc, :], start=(kc == 0), stop=(kc == 7)),
                       [k, "XeT"], [("ps", b0)])
                for kc in range(8):
                    pe(lambda e, kc=kc, t=t, pu_=pu_: e.matmul(pu_, lhsT=t[:, kc, 1, :], rhs=XeT[:, kc, :], start=(kc == 0), stop=(kc == 7)),
                       [k, "XeT"], [("ps", b0 + 1)])
                a2 = it % 2
                g_t, u_t, s_t = gs_[a2], us_[a2], sgm[a2]
                dve(lambda e, pg_=pg_, g_t=g_t, j=j, ex=ex: e.tensor_scalar(
                    out=g_t, in0=pg_, scalar1=bguT[:, j, ex:ex + 1], scalar2=7.0, op0=ALU.add, op1=ALU.min),
                    [("ps", b0), "bguT"], [("gs", a2)])
                act(lambda e, g_t=g_t, s_t=s_t: e.activation(out=s_t, in_=g_t, func=ACT.Sigmoid, scale=1.702),
                    [("gs", a2)], [("sgm", a2)])
                act(lambda e, pu_=pu_, u_t=u_t, j=j, ex=ex: e.activation(out=u_t, in_=pu_, func=ACT.Identity,
                                                                        bias=bguT[:, 8 + j, ex:ex + 1]),
                    [("ps", b0 + 1), "bguT"], [("us", a2)])
                dve(lambda e, u_t=u_t: e.tensor_scalar(out=u_t, in0=u_t, scalar1=7.0, scalar2=-7.0, op0=ALU.min, op1=ALU.max),
                    [("us", a2)], [("us", a2)])
                dve(lambda e, g_t=g_t, s_t=s_t: e.tensor_tensor(out=g_t, in0=g_t, in1=s_t, op=ALU.mult),
                    [("gs", a2), ("sgm", a2)], [("gs", a2)])
                dve(lambda e, g_t=g_t, u_t=u_t, j=j: e.scalar_tensor_tensor(out=hT[:, j, :], in0=u_t, scalar=1.0, in1=g_t,
                                                                           op0=ALU.add, op1=ALU.mult),
                    [("gs", a2), ("us", a2)], ["hT"])
                itc[0] += 1
                if j == 3 and ex + 1 < n_exp:
                    make_sel(ex + 1)
            return dn_tiles

        def down(ex, dn_tiles):
            for g in range(NG):
                for nh in range(2):
                    t, k = dn_tiles[nh]
                    pb_ = 6 + (g * 2 + nh) % 2
                    py_ = psb(pb_)
                    for kc in range(8):
                        pe(lambda e, kc=kc, t=t, g=g, py_=py_: e.matmul(
                            py_, lhsT=hT[:, kc, g * 128:(g + 1) * 128], rhs=t[:, kc, :], start=(kc == 0), stop=(kc == 7)),
                           [k, "hT"], [("ps", pb_)])
                    act(lambda e, g=g, nh=nh, py_=py_: e.activation(out=Yb[:, g, nh * 512:(nh + 1) * 512], in_=py_, func=ACT.Copy),
                        [("ps", pb_)], ["Yb"])

        def combine(ex):
            for i in range(NT):
                dve(lambda e, i=i, ex=ex: e.tensor_scalar(out=selw[:, i, :], in0=iot, scalar1=posm[:, i, ex:ex + 1],
                                                         scalar2=Wr[:, i, ex:ex + 1], op0=ALU.is_equal, op1=ALU.mult),
                    ["iot", ("posm", i), ("Wr", i)], ["selw"])
            TB = min(8, NT)
            for hb_ in range(NT // TB):
                tpv = pst(4 + hb_ % 2)[:, 0:TB, :]
                for i8 in range(TB):
                    i = hb_ * TB + i8
                    pe(lambda e, i=i, i8=i8, tpv=tpv: e.transpose(out=tpv[:, i8, :], in_=selw[:, i, :], identity=identb),
                       ["selw", "identb"], [("ps", 4 + hb_ % 2)])
                act(lambda e, hb_=hb_, tpv=tpv, TB=TB: e.activation(out=selwT[:, hb_ * TB:(hb_ + 1) * TB, :], in_=tpv, func=ACT.Copy),
                    [("ps", 4 + hb_ % 2)], ["selwT"])
            for i in range(NT):
                g = i // 4
                for nh in range(2):
                    pb_ = cit[0] % 4
                    pc_ = psb(pb_)
                    pe(lambda e, i=i, g=g, nh=nh, pc_=pc_: e.matmul(pc_, lhsT=selwT[:, i, :], rhs=Yb[:, g, nh * 512:(nh + 1) * 512],
                                                                    start=True, stop=True),
                       ["selwT", "Yb"], [("ps", pb_)])
                    if nh == 0:
                        dve(lambda e, i=i, nh=nh, pc_=pc_: e.tensor_tensor(out=x1[:, i, nh * 512:(nh + 1) * 512], in0=pc_,
                                                                           in1=x1[:, i, nh * 512:(nh + 1) * 512], op=ALU.add),
                            [("ps", pb_), ("x1h", i, nh)], [("x1h", i, nh)])
                    else:
                        ck = cit[0] % 4 // 2
                        tmp = ctmp[ck]
                        act(lambda e, tmp=tmp, pc_=pc_: e.activation(out=tmp, in_=pc_, func=ACT.Copy), [("ps", pb_)], [("ctmp", ck)])
                        pool(lambda e, i=i, nh=nh, tmp=tmp: e.tensor_tensor(out=x1[:, i, nh * 512:(nh + 1) * 512], in0=tmp,
                                                                            in1=x1[:, i, nh * 512:(nh + 1) * 512], op=ALU.add),
                             [("ctmp", ck), ("x1h", i, nh)], [("x1h", i, nh)])
                    cit[0] += 1

        make_sel(0)
        gather_pass(0, 0)
        gather_pass(0, 1)
        for ex in range(n_exp):
            dnt = gateup(ex)
            if ex + 1 < n_exp:
                gather_pass(ex + 1, 0)
            down(ex, dnt)
            if ex + 1 < n_exp:
                gather_pass(ex + 1, 1)
            combine(ex)
        if debug:
            for i in range(NT):
                dma(dbg["x2"][i * 128:(i + 1) * 128, :], x1[:, i, :], [("x1", i)], [], "dbg", final=True)
        P.barrier()
        A.reset(mD)

        wpg = A.alloc([128, 8, D], BF16)
        wpp = A.alloc([128, 2, D], BF16)
        hpT = [A.alloc([128, 8, 128], BF16) for _ in range(2)]
        hbs = [A.alloc([128, D], BF16) for _ in range(2)]
        sqs = A.alloc([128, D], F32)
        pf = [A.alloc([128, 256], F32) for _ in range(2)]
        pbf = [A.alloc([128, 256], BF16) for _ in range(2)]
        pT = [A.alloc([128, 2, 128], BF16) for _ in range(2)]
        sgp = [A.alloc([128, 512], F32) for _ in range(2)]
        ob = [A.alloc([128, D], F32) for _ in range(2)]
        dma(g_bc, g_ple.to_broadcast([128, D]), [], ["g_bc"], "c6")
        for hlf in range(2):
            load_w_cast(wpg[:, hlf * 4:(hlf + 1) * 4, :], w_ple_gate[hlf * 512:(hlf + 1) * 512, :].rearrange("(kc p) n -> p kc n", p=128),
                        ("wo", hlf), "wpg")
        load_w_cast(wpp, w_ple_proj.rearrange("(kc p) n -> p kc n", p=128), ("wb", 0), "wpp")
        for i in range(NT):
            a2 = i % 2
            src = x1[:, i, :]
            hb = hbs[a2]
            rms_rows(src, D, g_bc, hb, sqs, {"r": [("x1", i)], "s": "", "g": ["g_bc"], "w": [("hb", a2)]})
            tpb = a2
            tp = pst(tpb)
            for kc in range(8):
                pe(lambda e, kc=kc, hb=hb, tp=tp: e.transpose(out=tp[:, kc, :], in_=hb[:, kc * 128:(kc + 1) * 128], identity=identb),
                   [("hb", a2), "identb"], [("ps", tpb)])
            act(lambda e, a2=a2, tp=tp: e.activation(out=hpT[a2], in_=tp, func=ACT.Copy), [("ps", tpb)], [("hpT", a2)])
            dma(pf[a2], p_in[i * 128:(i + 1) * 128, :], [], [("pf", a2)], ("pf", a2))
            pool(lambda e, a2=a2: e.tensor_copy(out=pbf[a2], in_=pf[a2]), [("pf", a2)], [("pbf", a2)])
            tpb2 = 2 + a2
            tp2 = pst(tpb2)
            for cc in range(2):
                pe(lambda e, cc=cc, a2=a2, tp2=tp2: e.transpose(out=tp2[:, cc, :], in_=pbf[a2][:, cc * 128:(cc + 1) * 128], identity=identb),
                   [("pbf", a2), "identb"], [("ps", tpb2)])
            act(lambda e, a2=a2, tp2=tp2: e.activation(out=pT[a2], in_=tp2[:, 0:2, :], func=ACT.Copy), [("ps", tpb2)], [("pT", a2)])
            for nh in range(2):
                pgb = 4 + nh * 2
                pg_ = psb(pgb)
                pp_ = psb(pgb + 1)
                for kc in range(8):
                    pe(lambda e, kc=kc, a2=a2, nh=nh, pg_=pg_: e.matmul(pg_, lhsT=hpT[a2][:, kc, :], rhs=wpg[:, kc, nh * 512:(nh + 1) * 512],
                                                                        start=(kc == 0), stop=(kc == 7)),
                       [("hpT", a2), "wpg"], [("ps", pgb)])
                for cc in range(2):
                    pe(lambda e, cc=cc, a2=a2, nh=nh, pp_=pp_: e.matmul(pp_, lhsT=pT[a2][:, cc, :], rhs=wpp[:, cc, nh * 512:(nh + 1) * 512],
                                                                        start=(cc == 0), stop=(cc == 1)),
                       [("pT", a2), "wpp"], [("ps", pgb + 1)])
                act(lambda e, nh=nh, pg_=pg_: e.activation(out=sgp[nh], in_=pg_, func=ACT.Sigmoid), [("ps", pgb)], [("sgp", nh)])
                dve(lambda e, nh=nh, pp_=pp_: e.tensor_tensor(out=sgp[nh], in0=pp_, in1=sgp[nh], op=ALU.mult),
                    [("ps", pgb + 1), ("sgp", nh)], [("sgp", nh)])
                pool(lambda e, nh=nh, a2=a2, i=i: e.tensor_tensor(out=ob[a2][:, nh * 512:(nh + 1) * 512], in0=sgp[nh],
                                                                   in1=x1[:, i, nh * 512:(nh + 1) * 512], op=ALU.add),
                     [("sgp", nh), ("x1", i)], [("ob", a2)])
            dma(out[i * 128:(i + 1) * 128, :], ob[a2], [("ob", a2)], [], ("ob", a2), final=True)
        P.emit()
    return nc


def make_consts(S):
    pos = np.arange(S, dtype=np.float32)[:, None]
    invA = (10000.0 ** (-(np.arange(32, dtype=np.float32) / 32.0))).astype(np.float32)[None, :]
    invB = (10000.0 ** (-(np.arange(16, dtype=np.float32) / 16.0))).astype(np.float32)[None, :]
    angA = (pos * invA).astype(np.float32)
    angB = (pos * invB).astype(np.float32)
    tri = np.where(np.arange(128)[:, None] > np.arange(128)[None, :], NEG, 0.0).astype(np.float32)
    return {
        "c_ident": np.eye(128, dtype=np.float32),
        "c_tri": tri,
        "c_triu": (np.arange(128)[:, None] < np.arange(128)[None, :]).astype(np.float32),
        "c_iota": np.arange(128, dtype=np.float32)[None, :],
        "c_cosA": np.cos(angA).astype(np.float32),
        "c_sinA": np.sin(angA).astype(np.float32),
        "c_cosB": np.cos(angB).astype(np.float32),
        "c_sinB": np.sin(angB).astype(np.float32),
    }


_PARAMS = ["g_mix", "w_in", "moba_q_norm", "moba_k_norm", "mla_q_lat_norm", "w_uq", "mla_kv_lat_norm", "w_ukv",
           "mla_q_norm", "mla_k_norm", "w_branch_a", "w_branch_b", "w_out", "g_ffn", "w_router", "b_router",
           "w_gate_up", "b_gate_up", "w_down", "b_down", "g_ple", "w_ple_gate", "w_ple_proj"]


def run(inputs, n_cores=8, debug=False, n_exp=NEXP):
    x = np.asarray(inputs["x"], dtype=np.float32)
    B, S, _ = x.shape
    assert B == n_cores
    p = np.asarray(inputs["p"], dtype=np.float32)[0]
    shared = {k: np.ascontiguousarray(np.asarray(inputs[k], dtype=np.float32)[0]) for k in _PARAMS}
    shared.update(make_consts(S))
    nc = build_nc(S, n_exp=n_exp, debug=debug)
    in_maps = []
    for b in range(B):
        m = dict(shared)
        m["x"] = np.ascontiguousarray(x[b])
        m["p"] = np.ascontiguousarray(p[b])
        in_maps.append(m)
    res = run_bass_kernel_spmd(nc, in_maps, core_ids=list(range(B)))
    return res


def kernel(**inputs):
    res = run(inputs)
    return np.stack([np.asarray(r["out"], dtype=np.float32) for r in res.results], axis=0)
```

```python
import contextlib
import math
import numpy as np
import concourse.bass as bass
import concourse.mybir as mybir
from concourse.bass_utils import run_bass_kernel_spmd

F32 = mybir.dt.float32
BF16 = mybir.dt.bfloat16
ALU = mybir.AluOpType
ACT = mybir.ActivationFunctionType
AX = mybir.AxisListType

ENGS = ("pe", "act", "dve", "pool", "sp")
EPS = 1e-6
NEG = -30000.0
D = 1024
NEXP = 32


class _Op:
    __slots__ = ("eng", "fn", "dma", "dsem", "deps", "sig", "has_dep")

    def __init__(self, eng, fn, dma, dsem):
        self.eng = eng
        self.fn = fn
        self.dma = dma
        self.dsem = dsem
        self.deps = []
        self.sig = None
        self.has_dep = False


class _Res:
    __slots__ = ("w", "readers")

    def __init__(self):
        self.w = None
        self.readers = {}


class Prog:
    def __init__(self, nc):
        self.nc = nc
        self.ops = []
        self.res = {}
        self.last_dma_on_sem = {}
        self.final_ops = []
        self.last_on_eng = {}
        self.pending_dma = {}
        self.rec = None

    def op(self, eng, fn, reads=(), writes=(), dma=False, dsem=None, final=False, extra_deps=()):
        if self.rec is not None:
            self.rec.append((eng, fn, tuple(reads), tuple(writes), dma, dsem, final))
            return None
        o = _Op(eng, fn, dma, dsem)
        deps = {}

        def add(d, raw):
            if d is None or d is o:
                return
            if (not d.dma) and (not dma) and d.eng == eng:
                if not raw:
                    return
                if eng == "pe":
                    return
            deps[id(d)] = d

        for r in reads:
            st = self.res.get(r)
            if st is not None:
                add(st.w, True)
        for w in writes:
            st = self.res.get(w)
            if st is None:
                st = self.res[w] = _Res()
            add(st.w, False)
            for rd in st.readers.values():
                add(rd, False)
        for d in extra_deps:
            deps[id(d)] = d
        if dma:
            prev = self.last_dma_on_sem.get(dsem)
            if prev is not None:
                deps[id(prev)] = prev
            self.last_dma_on_sem[dsem] = o
            self.pending_dma[dsem] = o
        o.deps = list(deps.values())
        for d in o.deps:
            d.has_dep = True
        rk = ("d", dsem) if dma else ("e", eng)
        for r in reads:
            st = self.res.get(r)
            if st is None:
                st = self.res[r] = _Res()
            st.readers[rk] = o
        for w in writes:
            st = self.res[w]
            st.w = o
            st.readers = {}
        if final:
            o.has_dep = True
            self.final_ops.append(o)
        if not dma:
            self.last_on_eng[eng] = o
        self.ops.append(o)
        return o

    def interleave(self, fn, idxs, group=2):
        idxs = list(idxs)
        for g0 in range(0, len(idxs), group):
            lists = []
            for i in idxs[g0:g0 + group]:
                self.rec = []
                fn(i)
                lists.append(self.rec)
                self.rec = None
            pos = [0] * len(lists)
            left = sum(len(l) for l in lists)
            while left:
                for k, l in enumerate(lists):
                    if pos[k] < len(l):
                        eng, f, r, w, dma, dsem, final = l[pos[k]]
                        pos[k] += 1
                        left -= 1
                        self.op(eng, f, reads=r, writes=w, dma=dma, dsem=dsem, final=final)

    def merge(self, lists):
        pos = [0] * len(lists)
        left = sum(len(l) for l in lists)
        while left:
            for k, l in enumerate(lists):
                if pos[k] < len(l):
                    eng, f, r, w, dma, dsem, final = l[pos[k]]
                    pos[k] += 1
                    left -= 1
                    self.op(eng, f, reads=r, writes=w, dma=dma, dsem=dsem, final=final)

    def barrier(self):
        tails = list(self.last_on_eng.values()) + list(self.pending_dma.values())
        for e in ENGS:
            self.op(e, lambda h: h.nop(), extra_deps=[t for t in tails])
        self.res = {}

    def emit(self):
        nc = self.nc
        with contextlib.ExitStack() as es:
            SEM_MAX = 4000
            nsig = {e: 0 for e in ENGS}
            for o in self.ops:
                if (not o.dma) and o.has_dep:
                    nsig[o.eng] += 1
            esem = {e: [es.enter_context(nc.semaphore("s_%s%d" % (e, k))) for k in range(nsig[e] // SEM_MAX + 1)]
                    for e in ENGS}
            dsems = {}
            for o in self.ops:
                if o.dma and o.dsem not in dsems:
                    dsems[o.dsem] = es.enter_context(nc.semaphore("d%d" % len(dsems)))
            ecount = {e: 0 for e in ENGS}
            dcount = {k: 0 for k in dsems}
            for o in self.ops:
                if o.dma:
                    dcount[o.dsem] += 16
                    o.sig = (dsems[o.dsem], dcount[o.dsem])
                elif o.has_dep:
                    c0 = ecount[o.eng]
                    ecount[o.eng] += 1
                    o.sig = (esem[o.eng][c0 // SEM_MAX], c0 % SEM_MAX + 1)
            block = es.enter_context(nc.Block())
            per_eng = {e: [o for o in self.ops if o.eng == e] for e in ENGS}
            final_ops = self.final_ops

            def run(e, handle):
                waited = {}
                for o in per_eng[e]:
                    for d in o.deps:
                        sem, val = d.sig
                        k = id(sem)
                        if waited.get(k, 0) < val:
                            handle.wait_ge(sem, val)
                            waited[k] = val
                    ins = o.fn(handle)
                    if o.sig is not None:
                        ins.then_inc(o.sig[0], 16 if o.dma else 1)
                if e == "sp":
                    for o in final_ops:
                        sem, val = o.sig
                        if waited.get(id(sem), 0) < val:
                            handle.wait_ge(sem, val)
                            waited[id(sem)] = val

            @block.tensor
            def _(h):
                run("pe", h)

            @block.scalar
            def _(h):
                run("act", h)

            @block.vector
            def _(h):
                run("dve", h)

            @block.gpsimd
            def _(h):
                run("pool", h)

            @block.sync
            def _(h):
                run("sp", h)


class Arena:
    def __init__(self, t, n):
        self.t = t
        self.n = n
        self.off = 0

    def alloc(self, shape, dtype=BF16):
        assert shape[0] == 128
        nel = 1
        for s in shape[1:]:
            nel *= s
        esz = 2 if dtype == BF16 else 4
        n16 = (nel * esz + 1) // 2
        n16 = (n16 + 31) // 32 * 32
        start = self.off
        self.off += n16
        assert self.off <= self.n, "arena overflow %d > %d" % (self.off, self.n)
        v = self.t[:, start:start + (nel * esz) // 2]
        if dtype != BF16:
            v = v.bitcast(dtype)
        if len(shape) > 2:
            names = " ".join("d%d" % k for k in range(len(shape) - 1))
            kw = {"d%d" % k: shape[k + 1] for k in range(len(shape) - 1)}
            v = v.rearrange("p (%s) -> p %s" % (names, names), **kw)
        return v

    def mark(self):
        return self.off

    def reset(self, m):
        self.off = m


def bc(ap, axis, shape):
    return ap.unsqueeze(axis).to_broadcast(list(shape))


def build_nc(S, n_exp=NEXP, debug=False):
    NT = S // 128
    NQ = S // 512
    NB = S // 256
    nc = bass.Bass("TRN2", target_bir_lowering=False)
    din = lambda name, shape: nc.dram_tensor(name, shape, F32, kind="ExternalInput").ap()
    x = din("x", [S, D])
    p_in = din("p", [S, 256])
    g_mix = din("g_mix", [1, D])
    w_in = din("w_in", [D, 4000])
    moba_q_norm = din("moba_q_norm", [1, 64])
    moba_k_norm = din("moba_k_norm", [1, 64])
    mla_q_lat_norm = din("mla_q_lat_norm", [1, 256])
    w_uq = din("w_uq", [256, 768])
    mla_kv_lat_norm = din("mla_kv_lat_norm", [1, 128])
    w_ukv = din("w_ukv", [128, 1024])
    mla_q_norm = din("mla_q_norm", [1, 96])
    mla_k_norm = din("mla_k_norm", [1, 96])
    w_branch_a = din("w_branch_a", [512, D])
    w_branch_b = din("w_branch_b", [512, D])
    w_out = din("w_out", [D, D])
    g_ffn = din("g_ffn", [1, D])
    w_router = din("w_router", [D, NEXP])
    b_router = din("b_router", [1, NEXP])
    w_gate_up = din("w_gate_up", [NEXP, D, 2048])
    b_gate_up = din("b_gate_up", [NEXP, 2048])
    w_down = din("w_down", [NEXP, D, D])
    b_down = din("b_down", [NEXP, D])
    g_ple = din("g_ple", [1, D])
    w_ple_gate = din("w_ple_gate", [D, D])
    w_ple_proj = din("w_ple_proj", [256, D])
    c_ident = din("c_ident", [128, 128])
    c_tri = din("c_tri", [128, 128])
    c_triu = din("c_triu", [128, 128])
    c_iota = din("c_iota", [1, 128])
    c_cosA = din("c_cosA", [S, 32])
    c_sinA = din("c_sinA", [S, 32])
    c_cosB = din("c_cosB", [S, 16])
    c_sinB = din("c_sinB", [S, 16])
    out = nc.dram_tensor("out", [S, D], F32, kind="ExternalOutput").ap()
    dbg = {}
    if debug:
        dbg["x1"] = nc.dram_tensor("dbg_x1", [S, D], F32, kind="ExternalOutput").ap()
        dbg["x2"] = nc.dram_tensor("dbg_x2", [S, D], F32, kind="ExternalOutput").ap()
        dbg["ya"] = nc.dram_tensor("dbg_ya", [512, S], F32, kind="ExternalOutput").ap()
        dbg["yb"] = nc.dram_tensor("dbg_yb", [512, S], F32, kind="ExternalOutput").ap()

    P = Prog(nc)
    AR_N = 99 * 1024 + 512
    with contextlib.ExitStack() as es:
        arena_t = es.enter_context(nc.sbuf_tensor("arena", [128, AR_N], BF16))
        PS = es.enter_context(nc.psum_tensor("ps", [128, 8, 512], F32))
        A = Arena(arena_t, AR_N)

        def psb(b):
            return PS[:, b, :]

        def pst(b):
            return PS[:, b, :].bitcast(BF16).rearrange("p (a b) -> p a b", a=8)

        dve = lambda fn, r, w: P.op("dve", fn, reads=r, writes=w)
        act = lambda fn, r, w: P.op("act", fn, reads=r, writes=w)
        pool = lambda fn, r, w: P.op("pool", fn, reads=r, writes=w)
        pe = lambda fn, r, w: P.op("pe", fn, reads=r, writes=w)
        _dq = [0]

        def dma(out_ap, in_ap, r, w, key, q=None, final=False):
            if q is None:
                q = "sp"
            return P.op(q, lambda e: e.dma_start(out=out_ap, in_=in_ap), reads=r, writes=w, dma=True,
                        dsem=key, final=final)

        def dma_cast(out_ap, in_ap, r, w, key):
            return P.op("pool", lambda e: e.dma_start(out=out_ap, in_=in_ap), reads=r, writes=w, dma=True, dsem=key)

        identf = A.alloc([128, 128], F32)
        identb = A.alloc([128, 128], BF16)
        g_bc = A.alloc([128, D], F32)
        stat = A.alloc([128, 64], F32)
        tmpf = A.alloc([128, 128], F32)
        m0 = A.mark()
        trib = A.alloc([128, 128], BF16)
        cosA = A.alloc([128, NT, 32], F32)
        sinA = A.alloc([128, NT, 32], F32)
        cosB = A.alloc([128, NT, 16], F32)
        sinB = A.alloc([128, NT, 16], F32)
        gqa = A.alloc([128, 64], F32)
        gka = A.alloc([128, 64], F32)
        gql = A.alloc([128, 256], F32)
        gkvl = A.alloc([128, 128], F32)
        gqb = A.alloc([128, 96], F32)
        gkb = A.alloc([128, 96], F32)

        dma(identf, c_ident, [], ["identf"], "c0")
        dma(tmpf, c_tri, [], ["tmpf"], "c1")
        dve(lambda e: e.tensor_copy(out=identb, in_=identf), ["identf"], ["identb"])
        dve(lambda e: e.tensor_copy(out=trib, in_=tmpf), ["tmpf"], ["trib"])
        dma(cosA, c_cosA.rearrange("(t p) d -> p t d", p=128), [], ["cosA"], "c2")
        dma(sinA, c_sinA.rearrange("(t p) d -> p t d", p=128), [], ["sinA"], "c3")
        dma(cosB, c_cosB.rearrange("(t p) d -> p t d", p=128), [], ["cosB"], "c4")
        dma(sinB, c_sinB.rearrange("(t p) d -> p t d", p=128), [], ["sinB"], "c5")
        dma(g_bc, g_mix.to_broadcast([128, D]), [], ["g_bc"], "c6")
        dma(gqa, moba_q_norm.to_broadcast([128, 64]), [], ["gqa"], "c7")
        dma(gka, moba_k_norm.to_broadcast([128, 64]), [], ["gka"], "c8")
        dma(gql, mla_q_lat_norm.to_broadcast([128, 256]), [], ["gql"], "c9")
        dma(gkvl, mla_kv_lat_norm.to_broadcast([128, 128]), [], ["gkvl"], "c10")
        dma(gqb, mla_q_norm.to_broadcast([128, 96]), [], ["gqb"], "c11")
        dma(gkb, mla_k_norm.to_broadcast([128, 96]), [], ["gkb"], "c12")

        ctr = [0]

        def uid():
            ctr[0] += 1
            return ctr[0]

        def rms_rows(src, Dw, gt, dst, sq, key, u=None):
            if u is None:
                u = uid() % 4
            ss = stat[:, 2 * u:2 * u + 1]
            rs = stat[:, 2 * u + 1:2 * u + 2]
            sn = "stat%d" % u
            act(lambda e: e.activation(out=sq, in_=src, func=ACT.Square, scale=1.0 / math.sqrt(Dw), accum_out=ss),
                key["r"], ["sq" + key["s"], sn])
            dve(lambda e: e.tensor_scalar(out=rs, in0=ss, scalar1=EPS, scalar2=None, op0=ALU.add), [sn], [sn + "a"])
            act(lambda e: e.activation(out=rs, in_=rs, func=ACT.Sqrt), [sn + "a"], [sn + "b"])
            dve(lambda e: e.reciprocal(out=rs, in_=rs), [sn + "b"], [sn + "c"])
            dve(lambda e: e.scalar_tensor_tensor(out=dst, in0=src, scalar=rs, in1=gt, op0=ALU.mult, op1=ALU.mult),
                key["r"] + [sn + "c"] + key["g"], key["w"])

        nrc = [0]

        def norm_rope(src3, H, Dh, gt, gkey, rd, cos_t, sin_t, dst3, wk, rkeys, wkeys, si=None):
            if si is None:
                si = nrc[0] % len(wk)
                nrc[0] += 1
            sq, qn, t1, t2, st_ = wk[si]
            R = lambda nm: "nr_%s%d" % (nm, si)
            sq3 = sq[:, 0:H * Dh].rearrange("p (h d) -> p h d", h=H)
            qn3 = qn[:, 0:H * Dh].rearrange("p (h d) -> p h d", h=H)
            ss = st_[:, 0:H]
            rs = st_[:, 8:8 + H]
            act(lambda e: e.activation(out=sq3, in_=src3, func=ACT.Square), rkeys, [R("sq")])
            dve(lambda e: e.tensor_reduce(out=ss, in_=sq3, axis=AX.X, op=ALU.add), [R("sq")], [R("ss")])
            dve(lambda e: e.tensor_scalar(out=rs, in0=ss, scalar1=1.0 / Dh, scalar2=EPS, op0=ALU.mult, op1=ALU.add),
                [R("ss")], [R("rs0")])
            act(lambda e: e.activation(out=rs, in_=rs, func=ACT.Sqrt), [R("rs0")], [R("rs1")])
            dve(lambda e: e.reciprocal(out=rs, in_=rs), [R("rs1")], [R("rs")])
            dve(lambda e: e.tensor_tensor(out=qn3, in0=src3, in1=bc(rs, 2, [128, H, Dh]), op=ALU.mult),
                rkeys + [R("rs")], [R("qn")])
            pool(lambda e: e.tensor_tensor(out=qn3, in0=qn3, in1=bc(gt, 1, [128, H, Dh]), op=ALU.mult),
                 [R("qn"), gkey], [R("qn")])
            nd = Dh - rd
            hf = rd // 2
            if nd > 0:
                act(lambda e: e.activation(out=dst3[:, :, 0:nd], in_=qn3[:, :, 0:nd], func=ACT.Copy), [R("qn")], wkeys)
            x1_ = qn3[:, :, nd:nd + hf]
            x2_ = qn3[:, :, nd + hf:Dh]
            cb = bc(cos_t, 1, [128, H, hf])
            sb_ = bc(sin_t, 1, [128, H, hf])
            t13 = t1[:, 0:H * hf].rearrange("p (h d) -> p h d", h=H)
            t23 = t2[:, 0:H * hf].rearrange("p (h d) -> p h d", h=H)
            t33 = t1[:, 256:256 + H * hf].rearrange("p (h d) -> p h d", h=H)
            t43 = t2[:, 256:256 + H * hf].rearrange("p (h d) -> p h d", h=H)
            dve(lambda e: e.tensor_tensor(out=t13, in0=x1_, in1=cb, op=ALU.mult), [R("qn")], [R("t1")])
            pool(lambda e: e.tensor_tensor(out=t23, in0=x2_, in1=sb_, op=ALU.mult), [R("qn")], [R("t2")])
            dve(lambda e: e.tensor_tensor(out=t33, in0=x2_, in1=cb, op=ALU.mult), [R("qn")], [R("t3")])
            pool(lambda e: e.tensor_tensor(out=t43, in0=x1_, in1=sb_, op=ALU.mult), [R("qn")], [R("t4")])
            dve(lambda e: e.tensor_tensor(out=dst3[:, :, nd:nd + hf], in0=t13, in1=t23, op=ALU.subtract),
                [R("t1"), R("t2")], wkeys)
            dve(lambda e: e.tensor_tensor(out=dst3[:, :, nd + hf:Dh], in0=t33, in1=t43, op=ALU.add),
                [R("t3"), R("t4")], wkeys)

        def load_w_cast(dst, src, key, rk):
            return dma_cast(dst, src, [], [rk], key)

        def norm_transpose(i, src, xT_dst, hb, sq, tpb, rsrc):
            rms_rows(src, D, g_bc, hb, sq, {"r": rsrc, "s": "", "g": ["g_bc"], "w": [("hb", i % 2)]})
            tp = pst(tpb)
            for kc in range(8):
                pe(lambda e, kc=kc: e.transpose(out=tp[:, kc, :], in_=hb[:, kc * 128:(kc + 1) * 128], identity=identb),
                   [("hb", i % 2), "identb"], [("ps", tpb)])
            act(lambda e: e.activation(out=xT_dst[:, :, i * 128:(i + 1) * 128], in_=tp, func=ACT.Copy),
                [("ps", tpb)], [("xT", i)])

        hnT = A.alloc([128, 8, S], BF16)
        yTa = A.alloc([128, 4, S], BF16)
        yTb = A.alloc([128, 4, S], BF16)
        mA = A.mark()
        xb = [A.alloc([128, D], F32) for _ in range(2)]
        hbs = [A.alloc([128, D], BF16) for _ in range(2)]
        sqs = A.alloc([128, D], F32)
        for i in range(NT):
            dma(xb[i % 2], x[i * 128:(i + 1) * 128, :], [], [("xb", i % 2)], ("xb", i % 2))
            norm_transpose(i, xb[i % 2], hnT, hbs[i % 2], sqs, i % 2, [("xb", i % 2)])
        P.barrier()
        A.reset(mA)

        qT = A.alloc([128, 4, S], BF16)
        kT = A.alloc([128, 4, S], BF16)
        vaug = A.alloc([128, NT, 4, 65], BF16)
        wbuf = A.alloc([128, 8, 1024], BF16)
        qs = [A.alloc([128, 4, 96], BF16) for _ in range(2)]
        ks = [A.alloc([128, 4, 96], BF16) for _ in range(2)]
        wk = [tuple([A.alloc([128, 512], F32) for _ in range(4)] + [A.alloc([128, 16], F32)]) for _ in range(4)]
        kmT = A.alloc([128, 4, 8], BF16)
        kmF = A.alloc([128, 4, 8], F32)
        gsb2 = [A.alloc([128, 4, 8], F32) for _ in range(2)]
        cmpb2 = [A.alloc([128, 4, 8, 8], F32) for _ in range(2)]
        rank2 = [A.alloc([128, 4, 8], F32) for _ in range(2)]
        pbuf = [A.alloc([128, 512], BF16) for _ in range(2)]
        ytok = [A.alloc([128, 4, 256], BF16) for _ in range(2)]
        rec = A.alloc([128, 8], F32)
        latb2 = [A.alloc([128, 384], BF16) for _ in range(2)]
        latT2 = [A.alloc([128, 3, 128], BF16) for _ in range(2)]
        kcat2 = [A.alloc([128, 4, 96], F32) for _ in range(2)]
        kpes2 = [A.alloc([128, 32], F32) for _ in range(2)]
        sql2 = [A.alloc([128, 384], F32) for _ in range(2)]

        pool(lambda e: e.memset(vaug[:, :, :, 64:65], 1.0), [], ["vaug_ones"])
        pool(lambda e: e.memset(qT, 0.0), [], ["qT"])
        pool(lambda e: e.memset(kT, 0.0), [], ["kT"])

        def attention(half, Kd, scale, yT):
            it = 0
            for Q in range(NQ):
                yt = ytok[Q % 2]
                for hh in range(4):
                    accb = 4 + (it % 2)
                    acc3 = psb(accb).rearrange("p (t c) -> p t c", t=4)
                    nj = 4 * Q + 4

                    def s_stage(j, Q=Q, hh=hh):
                        tlo = max(j, 4 * Q)
                        ncols = (4 * Q + 4 - tlo) * 128
                        qc0 = tlo * 128
                        sbk = 6 + (j % 2)
                        sps = psb(sbk)
                        diag = j >= 4 * Q
                        pe(lambda e: e.matmul(sps[:, 0:ncols], lhsT=kT[0:Kd, hh, j * 128:(j + 1) * 128],
                                              rhs=qT[0:Kd, hh, qc0:qc0 + ncols], start=True, stop=not diag),
                           ["qT", "kT"], [("ps", sbk)])
                        if diag:
                            pe(lambda e: e.matmul(sps[:, 0:128], lhsT=identb, rhs=trib, start=False, stop=True),
                               ["identb", "trib"], [("ps", sbk)])
                        pb = pbuf[j % 2]
                        act(lambda e: e.activation(out=pb[:, 0:ncols], in_=sps[:, 0:ncols], func=ACT.Exp, scale=scale),
                            [("ps", sbk)], [("pb", j % 2)])

                    def pv_stage(j, Q=Q, hh=hh, acc3=acc3, accb=accb):
                        tlo = max(j, 4 * Q)
                        pb = pbuf[j % 2]
                        for t in range(tlo, 4 * Q + 4):
                            c = (t - tlo) * 128
                            first = (j == 0 and t == tlo)
                            pe(lambda e, c=c, t=t, first=first: e.matmul(
                                acc3[:, t - 4 * Q, 0:65], lhsT=pb[:, c:c + 128], rhs=vaug[:, j, hh, :],
                                start=first, stop=(j == t), skip_group_check=True),
                               [("pb", j % 2), "vaug", "vaug_ones"], [("ps", accb)])

                    s_stage(0)
                    for j in range(nj):
                        if j + 1 < nj:
                            s_stage(j + 1)
                        pv_stage(j)
                    dve(lambda e, acc3=acc3: e.reciprocal(out=rec[:, 0:4], in_=acc3[:, :, 64]), [("ps", accb)], ["rec"])
                    dve(lambda e, acc3=acc3, yt=yt, hh=hh: e.tensor_tensor(
                        out=yt[:, :, hh * 64:(hh + 1) * 64], in0=acc3[:, :, 0:64],
                        in1=bc(rec[:, 0:4], 2, [128, 4, 64]), op=ALU.mult),
                        [("ps", accb), "rec"], [("yt", Q % 2)])
                    it += 1
                tpb = Q % 2
                tp = pst(tpb)
                for fc in range(2):
                    for tl in range(4):
                        pe(lambda e, fc=fc, tl=tl, yt=yt, tp=tp: e.transpose(
                            out=tp[:, fc * 4 + tl, :], in_=yt[:, tl, fc * 128:(fc + 1) * 128], identity=identb),
                           [("yt", Q % 2), "identb"], [("ps", tpb)])
                act(lambda e, tp=tp, Q=Q: e.activation(
                    out=yT[:, half * 2:half * 2 + 2, Q * 512:(Q + 1) * 512],
                    in_=tp.rearrange("p (f t) c -> p f (t c)", f=2), func=ACT.Copy),
                    [("ps", tpb)], ["yT"])

        def load_moba(half_):
            for part in range(3):
                c0 = part * 512 + half_ * 4 * 64
                load_w_cast(wbuf[:, :, part * 256:(part + 1) * 256],
                            w_in[:, c0:c0 + 256].rearrange("(kc p) n -> p kc n", p=128), ("wb", part), "wbuf")

        def load_mla(half_):
            h0_ = half_ * 4
            load_w_cast(wbuf[:, :, 0:416], w_in[:, 1536:1952].rearrange("(kc p) n -> p kc n", p=128), ("wb", 0), "wbuf")
            load_w_cast(wbuf[:, 0:2, 416:800], w_uq[:, h0_ * 96:(h0_ + 4) * 96].rearrange("(kc p) n -> p kc n", p=128),
                        ("wb", 1), "wbuf")
            load_w_cast(wbuf[:, 2, 416:928], w_ukv[:, h0_ * 128:(h0_ + 4) * 128], ("wb", 2), "wbuf")

        load_moba(0)
        for half in range(2):
            h0 = half * 4
            pool(lambda e: e.memset(qT[64:72, :, :], 0.0), [], ["qT"])
            for b in range(2):
                pool(lambda e, b=b: e.memset(qs[b][:, :, 64:72], 0.0), [], [("qs", b)])
            def k_tile(i):
                c = i // 2
                mb = i % 2
                pk = psb(mb)
                pv = psb(2 + mb)
                for kc in range(8):
                    pe(lambda e, kc=kc, i=i, pk=pk: e.matmul(pk[:, 0:256], lhsT=hnT[:, kc, i * 128:(i + 1) * 128],
                                                              rhs=wbuf[:, kc, 256:512], start=(kc == 0), stop=(kc == 7)),
                       ["wbuf"], [("ps", mb)])
                for kc in range(8):
                    pe(lambda e, kc=kc, i=i, pv=pv: e.matmul(pv[:, 0:256], lhsT=hnT[:, kc, i * 128:(i + 1) * 128],
                                                              rhs=wbuf[:, kc, 512:768], start=(kc == 0), stop=(kc == 7)),
                       ["wbuf"], [("ps", 2 + mb)])
                act(lambda e, i=i, pv=pv: e.activation(out=vaug[:, i, :, 0:64],
                                                        in_=pv[:, 0:256].rearrange("p (h d) -> p h d", h=4), func=ACT.Copy),
                    [("ps", 2 + mb)], ["vaug"])
                ksb = ks[i % 2]
                norm_rope(pk[:, 0:256].rearrange("p (h d) -> p h d", h=4), 4, 64, gka, "gka", 64,
                          cosA[:, i, :], sinA[:, i, :], ksb[:, :, 0:64], wk, [("ps", mb), "cosA", "sinA"], [("ks", i % 2)],
                          si=(i % 2) * 2)
                if c > 0:
                    pool(lambda e, ksb=ksb, c=c: e.memset(ksb[:, :, 64:64 + c], 0.0), [], [("ks", i % 2)])
                pool(lambda e, ksb=ksb, c=c: e.memset(ksb[:, :, 64 + c:65 + c], 1.0), [], [("ks", i % 2)])
                if c < 7:
                    pool(lambda e, ksb=ksb, c=c: e.memset(ksb[:, :, 65 + c:72], 0.0), [], [("ks", i % 2)])
                tpb = 4 + (i % 2)
                tp = pst(tpb)
                for hh in range(4):
                    pe(lambda e, hh=hh, ksb=ksb, tp=tp: e.transpose(out=tp[0:72, hh, :], in_=ksb[:, hh, 0:72], identity=identb),
                       [("ks", i % 2), "identb"], [("ps", tpb)])
                act(lambda e, i=i, tp=tp: e.activation(out=kT[0:72, :, i * 128:(i + 1) * 128], in_=tp[0:72, 0:4, :], func=ACT.Copy),
                    [("ps", tpb)], ["kT"])
            P.interleave(k_tile, range(NT))
            dve(lambda e: e.tensor_reduce(out=kmF[0:64, :, 0:NB],
                                          in_=kT[0:64, :, :].rearrange("p h (n k) -> p h n k", k=256),
                                          axis=AX.X, op=ALU.add), ["kT"], ["kmF"])
            dve(lambda e: e.tensor_scalar(out=kmT[0:64, :, 0:NB], in0=kmF[0:64, :, 0:NB], scalar1=1.0 / 256.0, scalar2=None,
                                          op0=ALU.mult), ["kmF"], ["kmT"])
            def q_tile(i):
                c = i // 2
                mb = i % 2
                gsb, cmpb, rank = gsb2[mb], cmpb2[mb], rank2[mb]
                G = lambda nm, mb=mb: "%s%d" % (nm, mb)
                pq = psb(mb)
                for kc in range(8):
                    pe(lambda e, kc=kc, i=i, pq=pq: e.matmul(pq[:, 0:256], lhsT=hnT[:, kc, i * 128:(i + 1) * 128],
                                                              rhs=wbuf[:, kc, 0:256], start=(kc == 0), stop=(kc == 7)),
                       ["wbuf"], [("ps", mb)])
                qsb = qs[i % 2]
                norm_rope(pq[:, 0:256].rearrange("p (h d) -> p h d", h=4), 4, 64, gqa, "gqa", 64,
                          cosA[:, i, :], sinA[:, i, :], qsb[:, :, 0:64], wk, [("ps", mb), "cosA", "sinA"], [("qs", i % 2)],
                          si=(i % 2) * 2)
                tpb = 4 + (i % 2)
                tp = pst(tpb)
                for hh in range(4):
                    pe(lambda e, hh=hh, qsb=qsb, tp=tp: e.transpose(out=tp[0:64, hh, :], in_=qsb[:, hh, 0:64], identity=identb),
                       [("qs", i % 2), "identb"], [("ps", tpb)])
                act(lambda e, i=i, tp=tp: e.activation(out=qT[0:64, :, i * 128:(i + 1) * 128], in_=tp[0:64, 0:4, :], func=ACT.Copy),
                    [("ps", tpb)], ["qT", ("qTi", i)])
                if c > 3:
                    gb = 2 + mb
                    gps = psb(gb)[:, 0:32].rearrange("p (h n) -> p h n", h=4)
                    for hh in range(4):
                        pe(lambda e, hh=hh, i=i, gps=gps, c=c: e.matmul(
                            gps[:, hh, 0:c], lhsT=qT[0:64, hh, i * 128:(i + 1) * 128], rhs=kmT[0:64, hh, 0:c],
                            start=(hh == 0), stop=(hh == 3), skip_group_check=True),
                           [("qTi", i), "kmT"], [("ps", gb)])
                    act(lambda e, gps=gps, c=c, gsb=gsb: e.activation(out=gsb[:, :, 0:c], in_=gps[:, :, 0:c], func=ACT.Copy),
                        [("ps", gb)], [G("gsb")])
                    gv = gsb[:, :, 0:c]
                    dve(lambda e, gv=gv, c=c, cmpb=cmpb: e.tensor_tensor(out=cmpb[:, :, 0:c, 0:c], in0=bc(gv, 2, [128, 4, c, c]),
                                                                        in1=bc(gv, 3, [128, 4, c, c]), op=ALU.is_gt),
                        [G("gsb")], [G("cmpb")])
                    dve(lambda e, c=c, cmpb=cmpb, rank=rank: e.tensor_reduce(out=rank[:, :, 0:c], in_=cmpb[:, :, 0:c, 0:c], axis=AX.X, op=ALU.add),
                        [G("cmpb")], [G("rank")])
                    dve(lambda e, c=c, qsb=qsb, rank=rank: e.tensor_scalar(out=qsb[:, :, 64:64 + c], in0=rank[:, :, 0:c], scalar1=2.5,
                                                                          scalar2=NEG, op0=ALU.is_gt, op1=ALU.mult),
                        [G("rank")], [("qs", i % 2)])
                    tpb2 = 6 + (i % 2)
                    tp2 = pst(tpb2)
                    for hh in range(4):
                        pe(lambda e, hh=hh, qsb=qsb, tp2=tp2: e.transpose(out=tp2[0:72, hh, :], in_=qsb[:, hh, 0:72], identity=identb),
                           [("qs", i % 2), "identb"], [("ps", tpb2)])
                    act(lambda e, i=i, tp2=tp2: e.activation(out=qT[64:72, :, i * 128:(i + 1) * 128], in_=tp2[64:72, 0:4, :],
                                                              func=ACT.Copy),
                        [("ps", tpb2)], ["qT", ("qTi", i)])
            P.interleave(q_tile, range(NT))
            if half == 0:
                load_moba(1)
            else:
                load_mla(0)
            attention(half, 128, 64 ** -0.5, yTa)

        for half in range(2):
            h0 = half * 4
            def mla_tile(i):
                mb = i % 2
                latb, latT, kcat, kpes, sql = latb2[mb], latT2[mb], kcat2[mb], kpes2[mb], sql2[mb]
                L = lambda nm, mb=mb: "%s%d" % (nm, mb)
                pl = psb(mb)
                for kc in range(8):
                    pe(lambda e, kc=kc, i=i, pl=pl: e.matmul(pl[:, 0:416], lhsT=hnT[:, kc, i * 128:(i + 1) * 128],
                                                              rhs=wbuf[:, kc, 0:416], start=(kc == 0), stop=(kc == 7)),
                       ["wbuf"], [("ps", mb)])
                rms_rows(pl[:, 0:256], 256, gql, latb[:, 0:256], sql[:, 0:256],
                         {"r": [("ps", mb)], "s": L("l"), "g": ["gql"], "w": [L("latb")]}, u=mb * 2)
                rms_rows(pl[:, 256:384], 128, gkvl, latb[:, 256:384], sql[:, 256:384],
                         {"r": [("ps", mb)], "s": L("l"), "g": ["gkvl"], "w": [L("latb")]}, u=mb * 2 + 1)
                act(lambda e, pl=pl, kpes=kpes: e.activation(out=kpes, in_=pl[:, 384:416], func=ACT.Copy), [("ps", mb)], [L("kpes")])
                tpb = 4 + (i % 2)
                tp = pst(tpb)
                for cc in range(3):
                    pe(lambda e, cc=cc, tp=tp, latb=latb: e.transpose(out=tp[:, cc, :], in_=latb[:, cc * 128:(cc + 1) * 128], identity=identb),
                       [L("latb"), "identb"], [("ps", tpb)])
                act(lambda e, tp=tp, latT=latT: e.activation(out=latT, in_=tp[:, 0:3, :], func=ACT.Copy), [("ps", tpb)], [L("latT")])
                pqb = psb(2 + mb)
                for cc in range(2):
                    pe(lambda e, cc=cc, pqb=pqb, latT=latT: e.matmul(pqb[:, 0:384], lhsT=latT[:, cc, :], rhs=wbuf[:, cc, 416:800],
                                                                      start=(cc == 0), stop=(cc == 1)),
                       [L("latT"), "wbuf"], [("ps", 2 + mb)])
                qsb = qs[i % 2]
                norm_rope(pqb[:, 0:384].rearrange("p (h d) -> p h d", h=4), 4, 96, gqb, "gqb", 32,
                          cosB[:, i, :], sinB[:, i, :], qsb, wk, [("ps", 2 + mb), "cosB", "sinB"], [("qs", i % 2)], si=mb * 2)
                pkv = psb(mb)
                pe(lambda e, pkv=pkv, latT=latT: e.matmul(pkv[:, 0:512], lhsT=latT[:, 2, :], rhs=wbuf[:, 2, 416:928], start=True, stop=True),
                   [L("latT"), "wbuf", L("kpes")], [("ps", mb)])
                pkv3 = pkv.rearrange("p (h d) -> p h d", h=4)
                act(lambda e, i=i, pkv3=pkv3: e.activation(out=vaug[:, i, :, 0:64], in_=pkv3[:, :, 64:128], func=ACT.Copy),
                    [("ps", mb)], ["vaug"])
                act(lambda e, pkv3=pkv3, kcat=kcat: e.activation(out=kcat[:, :, 0:64], in_=pkv3[:, :, 0:64], func=ACT.Copy),
                    [("ps", mb)], [L("kcat")])
                dve(lambda e, kcat=kcat, kpes=kpes: e.tensor_copy(out=kcat[:, :, 64:96], in_=bc(kpes, 1, [128, 4, 32])),
                    [L("kpes")], [L("kcat")])
                ksb = ks[i % 2]
                norm_rope(kcat, 4, 96, gkb, "gkb", 32, cosB[:, i, :], sinB[:, i, :], ksb, wk,
                          [L("kcat"), "cosB", "sinB"], [("ks", i % 2)], si=mb * 2 + 1)
                tpb2 = 6 + (i % 2)
                tp2 = pst(tpb2)
                for hh in range(4):
                    pe(lambda e, hh=hh, qsb=qsb, tp2=tp2: e.transpose(out=tp2[0:96, hh, :], in_=qsb[:, hh, :], identity=identb),
                       [("qs", i % 2), "identb"], [("ps", tpb2)])
                    pe(lambda e, hh=hh, ksb=ksb, tp2=tp2: e.transpose(out=tp2[0:96, 4 + hh, :], in_=ksb[:, hh, :], identity=identb),
                       [("ks", i % 2), "identb"], [("ps", tpb2)])
                act(lambda e, i=i, tp2=tp2: e.activation(out=qT[0:96, :, i * 128:(i + 1) * 128], in_=tp2[0:96, 0:4, :], func=ACT.Copy),
                    [("ps", tpb2)], ["qT"])
                act(lambda e, i=i, tp2=tp2: e.activation(out=kT[0:96, :, i * 128:(i + 1) * 128], in_=tp2[0:96, 4:8, :], func=ACT.Copy),
                    [("ps", tpb2)], ["kT"])
            P.interleave(mla_tile, range(NT))
            if half == 0:
                load_mla(1)
            attention(half, 128, 96 ** -0.5, yTb)

        if debug:
            for nm, yT in (("ya", yTa), ("yb", yTb)):
                P.barrier()
                for c4 in range(4):
                    for Q in range(NQ):
                        dbf = wk[0][0]
                        dve(lambda e, yT=yT, c4=c4, Q=Q, dbf=dbf: e.tensor_copy(out=dbf, in_=yT[:, c4, Q * 512:(Q + 1) * 512]),
                            [], ["dbf"])
                        dma(dbg[nm][c4 * 128:(c4 + 1) * 128, Q * 512:(Q + 1) * 512], dbf, ["dbf"], [], "dbg", final=True)
        P.barrier()
        A.reset(mA)

        mgT = A.alloc([128, 8, S], BF16)
        wg = [A.alloc([128, 8, 256], BF16) for _ in range(2)]
        wab = [A.alloc([128, 4, 256], BF16) for _ in range(2)]
        sg = [A.alloc([128, 512], F32) for _ in range(2)]
        m1 = [A.alloc([128, 512], F32) for _ in range(2)]
        it = 0
        def load_c(fc):
            w2 = fc % 2
            load_w_cast(wg[w2][:, :, 0:128], w_in[:, 1952 + fc * 128:1952 + (fc + 1) * 128].rearrange("(kc p) n -> p kc n", p=128),
                        ("wg", w2, 0), ("wg", w2))
            load_w_cast(wg[w2][:, :, 128:256], w_in[:, 2976 + fc * 128:2976 + (fc + 1) * 128].rearrange("(kc p) n -> p kc n", p=128),
                        ("wg", w2, 1), ("wg", w2))
            load_w_cast(wab[w2][:, :, 0:128], w_branch_a[:, fc * 128:(fc + 1) * 128].rearrange("(kc p) n -> p kc n", p=128),
                        ("wab", w2, 0), ("wab", w2))
            load_w_cast(wab[w2][:, :, 128:256], w_branch_b[:, fc * 128:(fc + 1) * 128].rearrange("(kc p) n -> p kc n", p=128),
                        ("wab", w2, 1), ("wab", w2))

        load_c(0)
        for fc in range(8):
            w2 = fc % 2
            if fc + 1 < 8:
                load_c(fc + 1)
            for tc in range(NQ):
                b0 = (it % 2) * 4
                cols = slice(tc * 512, (tc + 1) * 512)
                for br in range(2):
                    pg_ = psb(b0 + br * 2)
                    py_ = psb(b0 + br * 2 + 1)
                    yT = yTa if br == 0 else yTb
                    for kc in range(8):
                        pe(lambda e, kc=kc, pg_=pg_, br=br, w2=w2, cols=cols: e.matmul(
                            pg_, lhsT=wg[w2][:, kc, br * 128:(br + 1) * 128], rhs=hnT[:, kc, cols],
                            start=(kc == 0), stop=(kc == 7)), [("wg", w2)], [("ps", b0 + br * 2)])
                    for kc in range(4):
                        pe(lambda e, kc=kc, py_=py_, br=br, w2=w2, cols=cols, yT=yT: e.matmul(
                            py_, lhsT=wab[w2][:, kc, br * 128:(br + 1) * 128], rhs=yT[:, kc, cols],
                            start=(kc == 0), stop=(kc == 3)), [("wab", w2)], [("ps", b0 + br * 2 + 1)])
                    act(lambda e, pg_=pg_, br=br: e.activation(out=sg[br], in_=pg_, func=ACT.Sigmoid),
                        [("ps", b0 + br * 2)], [("sg", br)])
                    dve(lambda e, py_=py_, br=br: e.tensor_tensor(out=m1[br], in0=py_, in1=sg[br], op=ALU.mult),
                        [("ps", b0 + br * 2 + 1), ("sg", br)], [("m1", br)])
                pool(lambda e, fc=fc, cols=cols: e.tensor_tensor(out=mgT[:, fc, cols], in0=m1[0], in1=m1[1], op=ALU.add),
                     [("m1", 0), ("m1", 1)], ["mgT"])
                it += 1
        P.barrier()
        A.reset(m0)
        x1 = A.alloc([128, NT, D], F32)
        mD = A.mark()
        assert mD <= mA
        A.reset(mA)
        mgT2 = A.alloc([128, 8, S], BF16)
        wo = A.alloc([128, 8, D], BF16)
        for hlf in range(2):
            load_w_cast(wo[:, hlf * 4:(hlf + 1) * 4, :], w_out[hlf * 512:(hlf + 1) * 512, :].rearrange("(kc p) n -> p kc n", p=128),
                        ("wo", hlf), "wo")
        for i in range(NT):
            dma(x1[:, i, :], x[i * 128:(i + 1) * 128, :], [], [("x1", i)], ("x1l", i % 4))
            for nh in range(2):
                pb_ = (i * 2 + nh) % 8
                pp = psb(pb_)
                for kc in range(8):
                    pe(lambda e, kc=kc, i=i, nh=nh, pp=pp: e.matmul(pp, lhsT=mgT2[:, kc, i * 128:(i + 1) * 128],
                                                                    rhs=wo[:, kc, nh * 512:(nh + 1) * 512],
                                                                    start=(kc == 0), stop=(kc == 7)),
                       ["wo"], [("ps", pb_)])
                dve(lambda e, i=i, nh=nh, pp=pp: e.tensor_tensor(out=x1[:, i, nh * 512:(nh + 1) * 512], in0=pp,
                                                                 in1=x1[:, i, nh * 512:(nh + 1) * 512], op=ALU.add),
                    [("ps", pb_), ("x1", i)], [("x1", i)])
            if debug:
                dma(dbg["x1"][i * 128:(i + 1) * 128, :], x1[:, i, :], [("x1", i)], [], "dbg", final=True)
        P.barrier()
        A.reset(mD)

        NG = NT // 4
        hnk = A.alloc([128, NT, D], BF16)
        Wr = A.alloc([128, NT, NEXP], F32)
        posm = A.alloc([128, NT, NEXP], F32)
        bguT = A.alloc([128, 16, NEXP], F32)
        iot = A.alloc([128, 128], F32)
        mD2 = A.mark()
        bdn = A.alloc([128, D], F32)
        wr = A.alloc([128, 8, NEXP], F32)
        brb = A.alloc([128, NEXP], F32)
        maskb = A.alloc([128, NT, NEXP], BF16)
        triu = A.alloc([128, 128], BF16)
        onesb = A.alloc([128, 128], BF16)
        hf = A.alloc([128, D], F32)
        hfT = A.alloc([128, 8, 128], F32)
        sqs = A.alloc([128, D], F32)
        lgs = A.alloc([128, NEXP], F32)
        em = A.alloc([128, NEXP], F32)
        mk = A.alloc([128, NEXP], F32)
        m8 = A.alloc([128, 8], F32)
        bgl = A.alloc([128, 2048], F32)
        WrT = A.alloc([128, 128], F32)

        dma(g_bc, g_ffn.to_broadcast([128, D]), [], ["g_bc"], "c6")
        dma(wr, w_router.rearrange("(kc p) n -> p kc n", p=128), [], ["wr"], "c7")
        dma(brb, b_router.to_broadcast([128, NEXP]), [], ["brb"], "c8")
        dma(bgl[0:NEXP, :], b_gate_up, [], ["bgl"], "c9")
        dma(bdn[0:NEXP, :], b_down, [], ["bdn"], "c10")
        dma(iot, c_iota.to_broadcast([128, 128]), [], ["iot"], "c11")
        dma(tmpf, c_triu, [], ["tmpf"], "c12")
        dve(lambda e: e.tensor_copy(out=triu, in_=tmpf), ["tmpf"], ["triu"])
        pool(lambda e: e.memset(onesb, 1.0), [], ["onesb"])
        pbg = PS[:, 0:2, :].rearrange("p b n -> p (b n)")[:, 0:16 * NEXP].rearrange("p (j e) -> p j e", j=16)
        for j in range(16):
            pe(lambda e, j=j: e.transpose(out=pbg[:, j, :], in_=bgl[0:NEXP, j * 128:(j + 1) * 128], identity=identf[0:NEXP, 0:NEXP]),
               ["bgl", "identf"], [("ps", 0), ("ps", 1)])
        act(lambda e: e.activation(out=bguT, in_=pbg, func=ACT.Copy), [("ps", 0), ("ps", 1)], ["bguT"])

        hf2 = [hf, A.alloc([128, D], F32)]
        hfT2 = [hfT, A.alloc([128, 8, 128], F32)]
        lgs2 = [lgs, A.alloc([128, NEXP], F32)]
        em2 = [em, A.alloc([128, NEXP], F32)]
        mk2 = [mk, A.alloc([128, NEXP], F32)]
        m82 = [m8, A.alloc([128, 8], F32)]
        WrT2 = [WrT, A.alloc([128, 128], F32)]
        st2 = [stat, A.alloc([128, 64], F32)]

        def route_tile(i, sqs=sqs):
            p2 = i % 2
            hf, hfT, lgs, em, mk, m8, WrT, st = hf2[p2], hfT2[p2], lgs2[p2], em2[p2], mk2[p2], m82[p2], WrT2[p2], st2[p2]
            N = lambda nm, p2=p2: "%s_%d" % (nm, p2)
            B0 = 4 * p2
            src = x1[:, i, :]
            ss = st[:, 0:1]
            rs = st[:, 1:2]
            act(lambda e: e.activation(out=sqs, in_=src, func=ACT.Square, scale=1.0 / 32.0, accum_out=ss),
                [("x1", i)], ["sqs", N("stat")])
            dve(lambda e: e.tensor_scalar(out=rs, in0=ss, scalar1=EPS, scalar2=None, op0=ALU.add), [N("stat")], [N("stat2a")])
            act(lambda e: e.activation(out=rs, in_=rs, func=ACT.Sqrt), [N("stat2a")], [N("stat2b")])
            dve(lambda e: e.reciprocal(out=rs, in_=rs), [N("stat2b")], [N("stat2")])
            dve(lambda e: e.scalar_tensor_tensor(out=hf, in0=src, scalar=rs, in1=g_bc, op0=ALU.mult, op1=ALU.mult),
                [("x1", i), N("stat2"), "g_bc"], [N("hf")])
            act(lambda e: e.activation(out=hnk[:, i, :], in_=hf, func=ACT.Copy), [N("hf")], [("hnk", i)])
            pft = PS[:, B0:B0 + 2, :].rearrange("p b n -> p (b n)").rearrange("p (k c) -> p k c", k=8)
            for kc in range(8):
                pe(lambda e, kc=kc: e.transpose(out=pft[:, kc, :], in_=hf[:, kc * 128:(kc + 1) * 128], identity=identf),
                   [N("hf"), "identf"], [("ps", B0), ("ps", B0 + 1)])
            dve(lambda e: e.tensor_copy(out=hfT, in_=pft), [("ps", B0), ("ps", B0 + 1)], [N("hfT")])
            plg = psb(B0 + 2)
            for kc in range(8):
                pe(lambda e, kc=kc: e.matmul(plg[:, 0:NEXP], lhsT=hfT[:, kc, :], rhs=wr[:, kc, :],
                                             start=(kc == 0), stop=(kc == 7)),
                   [N("hfT"), "wr"], [("ps", B0 + 2)])
            dve(lambda e: e.tensor_tensor(out=lgs, in0=plg[:, 0:NEXP], in1=brb, op=ALU.add),
                [("ps", B0 + 2), "brb"], [N("lgs")])
            dve(lambda e: e.max(out=m8, in_=lgs), [N("lgs")], [N("m8")])
            dve(lambda e: e.tensor_scalar(out=mk, in0=lgs, scalar1=m8[:, 3:4], scalar2=None, op0=ALU.is_ge),
                [N("lgs"), N("m8")], [N("mk")])
            act(lambda e: e.activation(out=maskb[:, i, :], in_=mk, func=ACT.Copy), [N("mk")], [("maskb", i)])
            dve(lambda e: e.tensor_scalar(out=st[:, 2:3], in0=m8[:, 0:1], scalar1=-1.0, scalar2=None, op0=ALU.mult),
                [N("m8")], [N("negmax")])
            act(lambda e: e.activation(out=em, in_=lgs, func=ACT.Exp, bias=st[:, 2:3], scale=1.0),
                [N("lgs"), N("negmax")], [N("em")])
            dve(lambda e: e.tensor_tensor(out=em, in0=em, in1=mk, op=ALU.mult), [N("em"), N("mk")], [N("em2")])
            dve(lambda e: e.tensor_reduce(out=st[:, 3:4], in_=em, axis=AX.X, op=ALU.add), [N("em2")], [N("den")])
            dve(lambda e: e.reciprocal(out=st[:, 4:5], in_=st[:, 3:4]), [N("den")], [N("rden")])
            dve(lambda e: e.tensor_scalar(out=Wr[:, i, :], in0=em, scalar1=st[:, 4:5], scalar2=None, op0=ALU.mult),
                [N("em2"), N("rden")], [("Wr", i)])
            ppos = psb(B0 + 2)[:, 64:64 + NEXP]
            i0 = (i // 4) * 4
            for i2 in range(i0, i + 1):
                lhs = triu if i2 == i else onesb
                pe(lambda e, i2=i2, lhs=lhs: e.matmul(ppos, lhsT=lhs, rhs=maskb[:, i2, :], start=(i2 == i0), stop=(i2 == i)),
                   [("maskb", i2), "triu", "onesb"], [("ps", B0 + 2)])
            dve(lambda e: e.scalar_tensor_tensor(out=st[:, 32:64], in0=ppos, scalar=1.0, in1=mk,
                                                 op0=ALU.add, op1=ALU.mult), [("ps", B0 + 2), N("mk")], [N("posp")])
            dve(lambda e: e.tensor_scalar(out=posm[:, i, :], in0=st[:, 32:64], scalar1=-1.0, scalar2=None, op0=ALU.add),
                [N("posp")], [("posm", i)])
            pwt = psb(B0 + 3)
            pe(lambda e: e.transpose(out=pwt[0:NEXP, 0:128], in_=Wr[:, i, :], identity=identf),
               [("Wr", i), "identf"], [("ps", B0 + 3)])
            act(lambda e: e.activation(out=WrT[0:NEXP, :], in_=pwt[0:NEXP, 0:128], func=ACT.Copy), [("ps", B0 + 3)], [N("WrT")])
            for nh in range(2):
                pbb = psb(B0 + nh)
                pe(lambda e, nh=nh, pbb=pbb: e.matmul(pbb, lhsT=WrT[0:NEXP, :], rhs=bdn[0:NEXP, nh * 512:(nh + 1) * 512],
                                                       start=True, stop=True), [N("WrT"), "bdn"], [("ps", B0 + nh)])
                dve(lambda e, nh=nh, pbb=pbb: e.tensor_tensor(out=x1[:, i, nh * 512:(nh + 1) * 512], in0=pbb,
                                                               in1=x1[:, i, nh * 512:(nh + 1) * 512], op=ALU.add),
                    [("ps", B0 + nh), ("x1", i)], [("x1", i)])

        P.interleave(route_tile, range(NT))
        P.barrier()
        A.reset(mD2)
        SL = NG * 128
        wgu = [A.alloc([128, 8, 2, 128], BF16) for _ in range(4)]
        wdn = [A.alloc([128, 8, 512], BF16) for _ in range(2)]
        sel = [A.alloc([128, NT, 128], BF16) for _ in range(2)]
        XeT = A.alloc([128, 8, SL], BF16)
        hT = A.alloc([128, 8, SL], BF16)
        Yb = A.alloc([128, NG, D], BF16)
        gs_ = [A.alloc([128, SL], F32) for _ in range(2)]
        us_ = [A.alloc([128, SL], F32) for _ in range(2)]
        sgm = [A.alloc([128, SL], F32) for _ in range(2)]
        selw = A.alloc([128, NT, 128], BF16)
        selwT = A.alloc([128, NT, 128], BF16)
        issued = [0]
        PF = 3

        def issue_gu(upto):
            while issued[0] < min(upto, n_exp * 8):
                n = issued[0]
                ex_, j_ = divmod(n, 8)
                t_ = wgu[n % 4]
                k_ = ("wgu", n % 4)
                for u in range(2):
                    wsrc = w_gate_up[ex_][:, u * 1024 + j_ * 128:u * 1024 + (j_ + 1) * 128].rearrange("(kc p) n -> p kc n", p=128)
                    load_w_cast(t_[:, :, u, :], wsrc, ("wgu", n % 4, u), k_)
                issued[0] += 1

        def make_sel(ex_):
            sl_ = sel[ex_ % 2]
            for i in range(NT):
                dve(lambda e, i=i, sl_=sl_, ex_=ex_: e.tensor_scalar(out=sl_[:, i, :], in0=iot, scalar1=posm[:, i, ex_:ex_ + 1],
                                                                    scalar2=None, op0=ALU.is_equal),
                    ["iot", ("posm", i)], [("sel", ex_ % 2)])

        cit = [0]
        itc = [0]
        npd_ = [0]

        def gather_pass(ex, ps_):
            sl = sel[ex % 2]
            for f4 in range(4):
                f = ps_ * 4 + f4
                pf_ = psb(f4)
                for g in range(NG):
                    for i in range(g * 4, g * 4 + 4):
                        pe(lambda e, i=i, f=f, g=g, pf_=pf_, sl=sl: e.matmul(
                            pf_[:, g * 128:(g + 1) * 128], lhsT=hnk[:, i, f * 128:(f + 1) * 128], rhs=sl[:, i, :],
                            start=(i == g * 4), stop=(i == g * 4 + 3), skip_group_check=True),
                           [("sel", ex % 2)], [("ps", f4)])
            act(lambda e, ps_=ps_: e.activation(out=XeT[:, ps_ * 4:(ps_ + 1) * 4, :], in_=PS[:, 0:4, 0:SL], func=ACT.Copy),
                [("ps", 0), ("ps", 1), ("ps", 2), ("ps", 3)], ["XeT"])

        def gateup(ex):
            issue_gu(ex * 8 + PF)
            dn_tiles = []
            for nh in range(2):
                t = wdn[npd_[0] % 2]
                k = ("wdn", npd_[0] % 2)
                load_w_cast(t, w_down[ex][:, nh * 512:(nh + 1) * 512].rearrange("(kc p) n -> p kc n", p=128), k, k)
                dn_tiles.append((t, k))
                npd_[0] += 1
            for j in range(8):
                n = ex * 8 + j
                issue_gu(n + PF)
                t = wgu[n % 4]
                k = ("wgu", n % 4)
                it = itc[0]
                b0 = 4 + (it % 2) * 2
                pg_ = psb(b0)[:, 0:SL]
                pu_ = psb(b0 + 1)[:, 0:SL]
                for kc in range(8):
                    pe(lambda e, kc=kc, t=t, pg_=pg_: e.matmul(pg_, lhsT=t[:, kc, 0, :], rhs=XeT[:, kc, :], start=(kc == 0), stop=(kc == 7)),
                       [k, "XeT"], [("ps", b0)])
                for kc in range(8):
                    pe(lambda e, kc=kc, t=t, pu_=pu_: e.matmul(pu_, lhsT=t[:, kc, 1, :], rhs=XeT[:, kc, :], start=(kc == 0), stop=(kc == 7)),
                       [k, "XeT"], [("ps", b0 + 1)])
                a2 = it % 2
                g_t, u_t, s_t = gs_[a2], us_[a2], sgm[a2]
                dve(lambda e, pg_=pg_, g_t=g_t, j=j, ex=ex: e.tensor_scalar(
                    out=g_t, in0=pg_, scalar1=bguT[:, j, ex:ex + 1], scalar2=7.0, op0=ALU.add, op1=ALU.min),
                    [("ps", b0), "bguT"], [("gs", a2)])
                act(lambda e, g_t=g_t, s_t=s_t: e.activation(out=s_t, in_=g_t, func=ACT.Sigmoid, scale=1.702),
                    [("gs", a2)], [("sgm", a2)])
                act(lambda e, pu_=pu_, u_t=u_t, j=j, ex=ex: e.activation(out=u_t, in_=pu_, func=ACT.Identity,
                                                                        bias=bguT[:, 8 + j, ex:ex + 1]),
                    [("ps", b0 + 1), "bguT"], [("us", a2)])
                dve(lambda e, u_t=u_t: e.tensor_scalar(out=u_t, in0=u_t, scalar1=7.0, scalar2=-7.0, op0=ALU.min, op1=ALU.max),
                    [("us", a2)], [("us", a2)])
                dve(lambda e, g_t=g_t, s_t=s_t: e.tensor_tensor(out=g_t, in0=g_t, in1=s_t, op=ALU.mult),
                    [("gs", a2), ("sgm", a2)], [("gs", a2)])
                dve(lambda e, g_t=g_t, u_t=u_t, j=j: e.scalar_tensor_tensor(out=hT[:, j, :], in0=u_t, scalar=1.0, in1=g_t,
                                                                           op0=ALU.add, op1=ALU.mult),
                    [("gs", a2), ("us", a2)], ["hT"])
                itc[0] += 1
                if j == 3 and ex + 1 < n_exp:
                    make_sel(ex + 1)
            return dn_tiles

        def down(ex, dn_tiles):
            for g in range(NG):
                for nh in range(2):
                    t, k = dn_tiles[nh]
                    pb_ = 6 + (g * 2 + nh) % 2
                    py_ = psb(pb_)
                    for kc in range(8):
                        pe(lambda e, kc=kc, t=t, g=g, py_=py_: e.matmul(
                            py_, lhsT=hT[:, kc, g * 128:(g + 1) * 128], rhs=t[:, kc, :], start=(kc == 0), stop=(kc == 7)),
                           [k, "hT"], [("ps", pb_)])
                    act(lambda e, g=g, nh=nh, py_=py_: e.activation(out=Yb[:, g, nh * 512:(nh + 1) * 512], in_=py_, func=ACT.Copy),
                        [("ps", pb_)], ["Yb"])

        def combine(ex):
            for i in range(NT):
                dve(lambda e, i=i, ex=ex: e.tensor_scalar(out=selw[:, i, :], in0=iot, scalar1=posm[:, i, ex:ex + 1],
                                                         scalar2=Wr[:, i, ex:ex + 1], op0=ALU.is_equal, op1=ALU.mult),
                    ["iot", ("posm", i), ("Wr", i)], ["selw"])
            TB = min(8, NT)
            for hb_ in range(NT // TB):
                tpv = pst(2 + hb_ % 2)[:, 0:TB, :]
                for i8 in range(TB):
                    i = hb_ * TB + i8
                    pe(lambda e, i=i, i8=i8, tpv=tpv: e.transpose(out=tpv[:, i8, :], in_=selw[:, i, :], identity=identb),
                       ["selw", "identb"], [("ps", 2 + hb_ % 2)])
                act(lambda e, hb_=hb_, tpv=tpv, TB=TB: e.activation(out=selwT[:, hb_ * TB:(hb_ + 1) * TB, :], in_=tpv, func=ACT.Copy),
                    [("ps", 2 + hb_ % 2)], ["selwT"])
            for i in range(NT):
                g = i // 4
                for nh in range(2):
                    pb_ = cit[0] % 2
                    pc_ = psb(pb_)
                    pe(lambda e, i=i, g=g, nh=nh, pc_=pc_: e.matmul(pc_, lhsT=selwT[:, i, :], rhs=Yb[:, g, nh * 512:(nh + 1) * 512],
                                                                    start=True, stop=True),
                       ["selwT", "Yb"], [("ps", pb_)])
                    dve(lambda e, i=i, nh=nh, pc_=pc_: e.tensor_tensor(out=x1[:, i, nh * 512:(nh + 1) * 512], in0=pc_,
                                                                       in1=x1[:, i, nh * 512:(nh + 1) * 512], op=ALU.add),
                        [("ps", pb_), ("x1", i)], [("x1", i)])
                    cit[0] += 1

        make_sel(0)
        gather_pass(0, 0)
        gather_pass(0, 1)
        dnt = gateup(0)
        for ex in range(n_exp):
            if ex + 1 < n_exp:
                gather_pass(ex + 1, 0)
            down(ex, dnt)
            if ex + 1 < n_exp:
                gather_pass(ex + 1, 1)
                P.rec = []
                combine(ex)
                la = P.rec
                P.rec = []
                dnt = gateup(ex + 1)
                lb = P.rec
                P.rec = None
                P.merge([la, lb])
            else:
                combine(ex)
        if debug:
            for i in range(NT):
                dma(dbg["x2"][i * 128:(i + 1) * 128, :], x1[:, i, :], [("x1", i)], [], "dbg", final=True)
        P.barrier()
        A.reset(mD)

        wpg = A.alloc([128, 8, D], BF16)
        wpp = A.alloc([128, 2, D], BF16)
        hpT = [A.alloc([128, 8, 128], BF16) for _ in range(2)]
        hbs = [A.alloc([128, D], BF16) for _ in range(2)]
        sqs = A.alloc([128, D], F32)
        pf = [A.alloc([128, 256], F32) for _ in range(2)]
        pbf = [A.alloc([128, 256], BF16) for _ in range(2)]
        pT = [A.alloc([128, 2, 128], BF16) for _ in range(2)]
        sgp = [A.alloc([128, 512], F32) for _ in range(2)]
        ob = [A.alloc([128, D], F32) for _ in range(2)]
        dma(g_bc, g_ple.to_broadcast([128, D]), [], ["g_bc"], "c6")
        for hlf in range(2):
            load_w_cast(wpg[:, hlf * 4:(hlf + 1) * 4, :], w_ple_gate[hlf * 512:(hlf + 1) * 512, :].rearrange("(kc p) n -> p kc n", p=128),
                        ("wo", hlf), "wpg")
        load_w_cast(wpp, w_ple_proj.rearrange("(kc p) n -> p kc n", p=128), ("wb", 0), "wpp")
        for i in range(NT):
            a2 = i % 2
            src = x1[:, i, :]
            hb = hbs[a2]
            rms_rows(src, D, g_bc, hb, sqs, {"r": [("x1", i)], "s": "", "g": ["g_bc"], "w": [("hb", a2)]})
            tpb = a2
            tp = pst(tpb)
            for kc in range(8):
                pe(lambda e, kc=kc, hb=hb, tp=tp: e.transpose(out=tp[:, kc, :], in_=hb[:, kc * 128:(kc + 1) * 128], identity=identb),
                   [("hb", a2), "identb"], [("ps", tpb)])
            act(lambda e, a2=a2, tp=tp: e.activation(out=hpT[a2], in_=tp, func=ACT.Copy), [("ps", tpb)], [("hpT", a2)])
            dma(pf[a2], p_in[i * 128:(i + 1) * 128, :], [], [("pf", a2)], ("pf", a2))
            pool(lambda e, a2=a2: e.tensor_copy(out=pbf[a2], in_=pf[a2]), [("pf", a2)], [("pbf", a2)])
            tpb2 = 2 + a2
            tp2 = pst(tpb2)
            for cc in range(2):
                pe(lambda e, cc=cc, a2=a2, tp2=tp2: e.transpose(out=tp2[:, cc, :], in_=pbf[a2][:, cc * 128:(cc + 1) * 128], identity=identb),
                   [("pbf", a2), "identb"], [("ps", tpb2)])
            act(lambda e, a2=a2, tp2=tp2: e.activation(out=pT[a2], in_=tp2[:, 0:2, :], func=ACT.Copy), [("ps", tpb2)], [("pT", a2)])
            for nh in range(2):
                pgb = 4 + nh * 2
                pg_ = psb(pgb)
                pp_ = psb(pgb + 1)
                for kc in range(8):
                    pe(lambda e, kc=kc, a2=a2, nh=nh, pg_=pg_: e.matmul(pg_, lhsT=hpT[a2][:, kc, :], rhs=wpg[:, kc, nh * 512:(nh + 1) * 512],
                                                                        start=(kc == 0), stop=(kc == 7)),
                       [("hpT", a2), "wpg"], [("ps", pgb)])
                for cc in range(2):
                    pe(lambda e, cc=cc, a2=a2, nh=nh, pp_=pp_: e.matmul(pp_, lhsT=pT[a2][:, cc, :], rhs=wpp[:, cc, nh * 512:(nh + 1) * 512],
                                                                        start=(cc == 0), stop=(cc == 1)),
                       [("pT", a2), "wpp"], [("ps", pgb + 1)])
                act(lambda e, nh=nh, pg_=pg_: e.activation(out=sgp[nh], in_=pg_, func=ACT.Sigmoid), [("ps", pgb)], [("sgp", nh)])
                dve(lambda e, nh=nh, pp_=pp_: e.tensor_tensor(out=sgp[nh], in0=pp_, in1=sgp[nh], op=ALU.mult),
                    [("ps", pgb + 1), ("sgp", nh)], [("sgp", nh)])
                pool(lambda e, nh=nh, a2=a2, i=i: e.tensor_tensor(out=ob[a2][:, nh * 512:(nh + 1) * 512], in0=sgp[nh],
                                                                   in1=x1[:, i, nh * 512:(nh + 1) * 512], op=ALU.add),
                     [("sgp", nh), ("x1", i)], [("ob", a2)])
            dma(out[i * 128:(i + 1) * 128, :], ob[a2], [("ob", a2)], [], ("ob", a2), final=True)
        P.emit()
    return nc


def make_consts(S):
    pos = np.arange(S, dtype=np.float32)[:, None]
    invA = (10000.0 ** (-(np.arange(32, dtype=np.float32) / 32.0))).astype(np.float32)[None, :]
    invB = (10000.0 ** (-(np.arange(16, dtype=np.float32) / 16.0))).astype(np.float32)[None, :]
    angA = (pos * invA).astype(np.float32)
    angB = (pos * invB).astype(np.float32)
    tri = np.where(np.arange(128)[:, None] > np.arange(128)[None, :], NEG, 0.0).astype(np.float32)
    return {
        "c_ident": np.eye(128, dtype=np.float32),
        "c_tri": tri,
        "c_triu": (np.arange(128)[:, None] < np.arange(128)[None, :]).astype(np.float32),
        "c_iota": np.arange(128, dtype=np.float32)[None, :],
        "c_cosA": np.cos(angA).astype(np.float32),
        "c_sinA": np.sin(angA).astype(np.float32),
        "c_cosB": np.cos(angB).astype(np.float32),
        "c_sinB": np.sin(angB).astype(np.float32),
    }


_PARAMS = ["g_mix", "w_in", "moba_q_norm", "moba_k_norm", "mla_q_lat_norm", "w_uq", "mla_kv_lat_norm", "w_ukv",
           "mla_q_norm", "mla_k_norm", "w_branch_a", "w_branch_b", "w_out", "g_ffn", "w_router", "b_router",
           "w_gate_up", "b_gate_up", "w_down", "b_down", "g_ple", "w_ple_gate", "w_ple_proj"]


def run(inputs, n_cores=8, debug=False, n_exp=NEXP):
    x = np.asarray(inputs["x"], dtype=np.float32)
    B, S, _ = x.shape
    assert B == n_cores
    p = np.asarray(inputs["p"], dtype=np.float32)[0]
    shared = {k: np.ascontiguousarray(np.asarray(inputs[k], dtype=np.float32)[0]) for k in _PARAMS}
    shared.update(make_consts(S))
    nc = build_nc(S, n_exp=n_exp, debug=debug)
    in_maps = []
    for b in range(B):
        m = dict(shared)
        m["x"] = np.ascontiguousarray(x[b])
        m["p"] = np.ascontiguousarray(p[b])
        in_maps.append(m)
    res = run_bass_kernel_spmd(nc, in_maps, core_ids=list(range(B)))
    return res


def kernel(**inputs):
    res = run(inputs)
    return np.stack([np.asarray(r["out"], dtype=np.float32) for r in res.results], axis=0)
```
